# Optimizing a Trainium2 kernel written in Bass

```python
import math
import jax, jax.numpy as jnp
from jax import lax
import numpy as np

D_MODEL = 2048
BATCH = 4
SEQ = 8192
DEPTH = 2

HEAD_DIM = 64
HEADS_PER_MIXER = 8
N_MIXERS = 4
MIX_WIDTH = N_MIXERS * HEADS_PER_MIXER * HEAD_DIM
D_FF = 4 * D_MODEL
QBLK = 128
EPS = 1e-6
NEG = -1e30
BIG = 1e30

N_BUCKETS = 32
MAX_EXACT = 16
MAX_DISTANCE = 4096
N_BIAS_HEADS = 3 * HEADS_PER_MIXER

NSA_KV_GROUPS = 2
NSA_Q_PER_GROUP = HEADS_PER_MIXER // NSA_KV_GROUPS
NSA_KV_WIDTH = NSA_KV_GROUPS * HEAD_DIM
NSA_CMP_LEN = 32
NSA_CMP_STRIDE = 16
NSA_CMP_HIDDEN = 256
NSA_SEL_LEN = 64
NSA_TOP_N = 16
NSA_WINDOW = 512

MOBA_BLOCK = 256
MOBA_TOPK = 3

LONGNET_PATTERNS = ((128, 1), (512, 4), (2048, 16))

GW = HEADS_PER_MIXER * HEAD_DIM
PROJ_SIZES = (
    GW, NSA_KV_WIDTH, NSA_KV_WIDTH, NSA_KV_WIDTH, NSA_KV_WIDTH, NSA_KV_WIDTH, NSA_KV_WIDTH, 3 * HEADS_PER_MIXER,
    GW, GW, GW,
    GW, GW, GW, HEADS_PER_MIXER,
    GW, GW, GW,
)
PROJ_WIDTH = 5920

kernel_name = "hybrid_nsa_moba_fox_longnet_block"


def rmsnorm(x, g):
    xf = x.astype(jnp.float32)
    y = xf * lax.rsqrt(jnp.mean(xf * xf, axis=-1, keepdims=True) + EPS)
    return (y * g.astype(jnp.float32)).astype(x.dtype)


def t5_bucket(dist):
    dist = jnp.maximum(dist, 0)
    rel = jnp.log(jnp.maximum(dist, 1).astype(jnp.float32) / MAX_EXACT) / math.log(MAX_DISTANCE / MAX_EXACT)
    large = jnp.minimum(MAX_EXACT + (rel * (N_BUCKETS - MAX_EXACT)).astype(jnp.int32), N_BUCKETS - 1)
    return jnp.where(dist < MAX_EXACT, dist, large)


def masked_softmax(s, mask):
    s = jnp.where(mask, s, NEG)
    p = jax.nn.softmax(s, axis=-1)
    return jnp.where(mask, p, 0.0)


def merge_blocks(o):
    o = jnp.moveaxis(o, 0, -3)
    return o.reshape(o.shape[:-3] + (o.shape[-3] * o.shape[-2], o.shape[-1]))


def banded_attention(q, k, v, max_dist, dist_step, bias_tab):
    B, G, R, L, hd = q.shape
    nb = -(-L // QBLK)
    Lp = nb * QBLK
    nw = -(-max_dist // QBLK)
    q = jnp.pad(q, ((0, 0), (0, 0), (0, 0), (0, Lp - L), (0, 0)))
    kv_pad = ((0, 0), (0, 0), (nw * QBLK, Lp - L), (0, 0))
    kb = jnp.pad(k, kv_pad).reshape(B, G, nb + nw, QBLK, hd)
    vb = jnp.pad(v, kv_pad).reshape(B, G, nb + nw, QBLK, hd)
    blk_idx = jnp.arange(nb)[:, None] + jnp.arange(nw + 1)[None, :]
    kw = kb[:, :, blk_idx].reshape(B, G, nb, (nw + 1) * QBLK, hd)
    vw = vb[:, :, blk_idx].reshape(B, G, nb, (nw + 1) * QBLK, hd)
    qb = q.reshape(B, G, R, nb, QBLK, hd)
    s = jnp.einsum('bgrnqd,bgnkd->bgrnqk', qb, kw).astype(jnp.float32) * (hd ** -0.5)
    qpos = jnp.arange(Lp).reshape(nb, QBLK)
    kpos = (blk_idx[:, :, None] * QBLK + jnp.arange(QBLK)).reshape(nb, -1) - nw * QBLK
    dist = qpos[:, :, None] - kpos[:, None, :]
    mask = (dist >= 0) & (dist <= max_dist) & (kpos[:, None, :] >= 0)
    bias = jnp.moveaxis(bias_tab[t5_bucket(dist * dist_step)], -1, 0).reshape(G, R, nb, QBLK, -1)
    s = jnp.where(mask, s + bias, NEG)
    lse = jax.nn.logsumexp(s, axis=-1)
    p = jnp.exp(s - lse[..., None])
    o = jnp.einsum('bgrnqk,bgnkd->bgrnqd', p.astype(v.dtype), vw)
    o = o.reshape(B, G, R, Lp, hd)[:, :, :, :L]
    lse = lse.reshape(B, G, R, Lp)[..., :L]
    return o, lse


def nsa_compress(kx, pe, w1, w2):
    B, G, T, hd = kx.shape
    nc = (T - NSA_CMP_LEN) // NSA_CMP_STRIDE + 1
    idx = jnp.arange(nc)[:, None] * NSA_CMP_STRIDE + jnp.arange(NSA_CMP_LEN)[None, :]
    blk = kx[:, :, idx] + pe
    hmid = jax.nn.gelu(blk.reshape(B, G, nc, NSA_CMP_LEN * hd) @ w1)
    return hmid @ w2


def nsa_attention(q, k_cmp, v_cmp, k_slc, v_slc, k_win, v_win, gate_logits,
                  cmp_pe, phik_w1, phik_w2, phiv_w1, phiv_w2, bias_tab):
    B, G, R, T, hd = q.shape
    scale = hd ** -0.5
    kc = nsa_compress(k_cmp, cmp_pe, phik_w1, phik_w2)
    vc = nsa_compress(v_cmp, cmp_pe, phiv_w1, phiv_w2)
    nc = kc.shape[2]
    ns = T // NSA_SEL_LEN
    n_sel = min(NSA_TOP_N, ns)
    c_end = jnp.arange(nc) * NSA_CMP_STRIDE + NSA_CMP_LEN - 1
    s_start = jnp.arange(ns) * NSA_SEL_LEN
    overlap = ((c_end[:, None] - NSA_CMP_LEN + 1 < s_start[None, :] + NSA_SEL_LEN)
               & (c_end[:, None] >= s_start[None, :])).astype(jnp.float32)
    kbl = k_slc.reshape(B, G, ns, NSA_SEL_LEN, hd)
    vbl = v_slc.reshape(B, G, ns, NSA_SEL_LEN, hd)
    tab_gr = bias_tab.T.reshape(G, R, N_BUCKETS)
    b_i = jnp.arange(B)[:, None, None, None]
    g_i = jnp.arange(G)[None, :, None, None]
    g6 = jnp.arange(G)[None, :, None, None, None, None]
    r6 = jnp.arange(R)[None, None, :, None, None, None]
    blk_id = jnp.arange(ns)

    def block(i):
        t = i * QBLK + jnp.arange(QBLK)
        qb = lax.dynamic_slice_in_dim(q, i * QBLK, QBLK, axis=3)
        sc = jnp.einsum('bgrqd,bgcd->bgrqc', qb, kc).astype(jnp.float32) * scale
        dc = t[:, None] - c_end[None, :]
        sc = sc + jnp.moveaxis(bias_tab[t5_bucket(dc)], -1, 0).reshape(G, R, QBLK, nc)
        pc = masked_softmax(sc, dc >= 0)
        o_c = jnp.einsum('bgrqc,bgcd->bgrqd', pc.astype(vc.dtype), vc)
        imp = jnp.einsum('bgrqc,cn->bgqn', pc, overlap)
        cur = t // NSA_SEL_LEN
        forced = (blk_id[None, :] == 0) | (blk_id[None, :] == cur[:, None]) | (blk_id[None, :] == cur[:, None] - 1)
        valid = s_start[None, :] <= t[:, None]
        imp = jnp.where(forced, BIG, jnp.where(valid, imp, NEG))
        _, sel = lax.top_k(imp, n_sel)
        ks = kbl[b_i, g_i, sel]
        vs = vbl[b_i, g_i, sel]
        ss = jnp.einsum('bgrqd,bgqnkd->bgrqnk', qb, ks).astype(jnp.float32) * scale
        spos = sel[..., None] * NSA_SEL_LEN + jnp.arange(NSA_SEL_LEN)
        ds = t[None, None, :, None, None] - spos
        ss = ss + tab_gr[g6, r6, t5_bucket(ds)[:, :, None]]
        ss = ss.reshape(B, G, R, QBLK, n_sel * NSA_SEL_LEN)
        ps = masked_softmax(ss, (ds >= 0).reshape(B, G, 1, QBLK, n_sel * NSA_SEL_LEN))
        o_s = jnp.einsum('bgrqk,bgqkd->bgrqd', ps.astype(vs.dtype),
                         vs.reshape(B, G, QBLK, n_sel * NSA_SEL_LEN, hd))
        return o_c, o_s

    o_c, o_s = lax.map(block, jnp.arange(T // QBLK))
    o_c = merge_blocks(o_c)
    o_s = merge_blocks(o_s)
    o_w, _ = banded_attention(q, k_win, v_win, NSA_WINDOW - 1, 1, bias_tab)
    gates = jax.nn.sigmoid(gate_logits.astype(jnp.float32)).reshape(B, T, 3, G, R).transpose(2, 0, 3, 4, 1)[..., None]
    o = gates[0] * o_c + gates[1] * o_s + gates[2] * o_w
    return o.reshape(B, G * R, T, hd)


def moba_attention(q, k, v, bias_tab):
    B, H, T, hd = q.shape
    scale = hd ** -0.5
    nkb = -(-T // MOBA_BLOCK)
    pad = ((0, 0), (0, 0), (0, nkb * MOBA_BLOCK - T), (0, 0))
    kb = jnp.pad(k, pad).reshape(B, H, nkb, MOBA_BLOCK, hd)
    vb = jnp.pad(v, pad).reshape(B, H, nkb, MOBA_BLOCK, hd)
    kmean = jnp.mean(kb.astype(jnp.float32), axis=3).astype(k.dtype)
    n_top = min(MOBA_TOPK, nkb)
    tab_t = bias_tab.T
    b_i = jnp.arange(B)[:, None, None, None]
    h_i = jnp.arange(H)[None, :, None, None]
    h5 = jnp.arange(H)[None, :, None, None, None]

    def block(i):
        t = i * QBLK + jnp.arange(QBLK)
        cur = (i * QBLK) // MOBA_BLOCK
        qb = lax.dynamic_slice_in_dim(q, i * QBLK, QBLK, axis=2)
        gate = jnp.einsum('bhqd,bhnd->bhqn', qb, kmean).astype(jnp.float32)
        gate = jnp.where(jnp.arange(nkb) < cur, gate, NEG)
        _, sel = lax.top_k(gate, n_top)
        ks = kb[b_i, h_i, sel]
        vs = vb[b_i, h_i, sel]
        s_sel = jnp.einsum('bhqd,bhqnkd->bhqnk', qb, ks).astype(jnp.float32) * scale
        pos_sel = sel[..., None] * MOBA_BLOCK + jnp.arange(MOBA_BLOCK)
        s_sel = s_sel + tab_t[h5, t5_bucket(t[None, None, :, None, None] - pos_sel)]
        mask_sel = jnp.broadcast_to((sel < cur)[..., None], pos_sel.shape)
        k_own = lax.dynamic_index_in_dim(kb, cur, axis=2, keepdims=False)
        v_own = lax.dynamic_index_in_dim(vb, cur, axis=2, keepdims=False)
        d_own = t[:, None] - (cur * MOBA_BLOCK + jnp.arange(MOBA_BLOCK))[None, :]
        s_own = jnp.einsum('bhqd,bhkd->bhqk', qb, k_own).astype(jnp.float32) * scale + tab_t[:, t5_bucket(d_own)]
        n_sel_keys = n_top * MOBA_BLOCK
        s = jnp.concatenate([s_sel.reshape(B, H, QBLK, n_sel_keys), s_own], axis=-1)
        mask = jnp.concatenate([mask_sel.reshape(B, H, QBLK, n_sel_keys),
                                jnp.broadcast_to(d_own >= 0, (B, H, QBLK, MOBA_BLOCK))], axis=-1)
        p = masked_softmax(s, mask).astype(v.dtype)
        return (jnp.einsum('bhqk,bhqkd->bhqd', p[..., :n_sel_keys], vs.reshape(B, H, QBLK, n_sel_keys, hd))
                + jnp.einsum('bhqk,bhkd->bhqd', p[..., n_sel_keys:], v_own))

    return merge_blocks(lax.map(block, jnp.arange(T // QBLK)))


def fox_attention(q, k, v, log_f):
    B, H, T, hd = q.shape
    scale = hd ** -0.5
    c = jnp.cumsum(log_f, axis=-1)
    kpos = jnp.arange(T)

    def block(i):
        t = i * QBLK + jnp.arange(QBLK)
        qb = lax.dynamic_slice_in_dim(q, i * QBLK, QBLK, axis=2)
        cq = lax.dynamic_slice_in_dim(c, i * QBLK, QBLK, axis=2)
        s = jnp.einsum('bhqd,bhkd->bhqk', qb, k).astype(jnp.float32) * scale + (cq[..., None] - c[:, :, None, :])
        p = masked_softmax(s, kpos[None, :] <= t[:, None])
        return jnp.einsum('bhqk,bhkd->bhqd', p.astype(v.dtype), v)

    return merge_blocks(lax.map(block, jnp.arange(T // QBLK)))


def dilated_attention(q, k, v, bias_tab):
    B, H, T, hd = q.shape
    outs, lses = [], []
    for window, dil in LONGNET_PATTERNS:
        L = T // dil

        def fold(z):
            return z.reshape(B, H, L, dil, hd).transpose(0, 3, 1, 2, 4).reshape(B * dil, H, L, hd)

        o, lse = banded_attention(fold(q)[:, :, None], fold(k), fold(v), window // dil, dil, bias_tab)
        outs.append(o[:, :, 0].reshape(B, dil, H, L, hd).transpose(0, 2, 3, 1, 4).reshape(B, H, T, hd))
        lses.append(lse[:, :, 0].reshape(B, dil, H, L).transpose(0, 2, 3, 1).reshape(B, H, T))
    w = jax.nn.softmax(jnp.stack(lses, axis=0), axis=0)
    out = w[0][..., None] * outs[0] + w[1][..., None] * outs[1] + w[2][..., None] * outs[2]
    return out.astype(q.dtype)


def hybrid_mixer(h, w_in, w_out, cmp_pe, phik_w1, phik_w2, phiv_w1, phiv_w2, fox_bias, rel_bias):
    B, T, _ = h.shape
    proj = h @ w_in
    splits = [int(s) for s in np.cumsum(PROJ_SIZES)[:-1]]
    (qa, kca, vca, ksa, vsa, kwa, vwa, ga,
     qb, kb, vb,
     qc, kc, vc, fc,
     qd, kd, vd) = jnp.split(proj, splits, axis=-1)

    def heads(z):
        n = z.shape[-1] // HEAD_DIM
        return z.reshape(B, T, n, HEAD_DIM).transpose(0, 2, 1, 3)

    hm = HEADS_PER_MIXER
    qa_g = heads(qa).reshape(B, NSA_KV_GROUPS, NSA_Q_PER_GROUP, T, HEAD_DIM)
    o_a = nsa_attention(qa_g, heads(kca), heads(vca), heads(ksa), heads(vsa), heads(kwa), heads(vwa), ga,
                        cmp_pe, phik_w1, phik_w2, phiv_w1, phiv_w2, rel_bias[:, 0:hm])
    o_b = moba_attention(heads(qb), heads(kb), heads(vb), rel_bias[:, hm:2 * hm])
    log_f = jax.nn.log_sigmoid((fc + fox_bias).astype(jnp.float32)).transpose(0, 2, 1)
    o_c = fox_attention(heads(qc), heads(kc), heads(vc), log_f)
    o_d = dilated_attention(heads(qd), heads(kd), heads(vd), rel_bias[:, 2 * hm:3 * hm])
    o = jnp.concatenate([o_a.astype(h.dtype), o_b.astype(h.dtype), o_c.astype(h.dtype), o_d.astype(h.dtype)], axis=1)
    o = o.transpose(0, 2, 1, 3).reshape(B, T, MIX_WIDTH)
    return o @ w_out


def setup_inputs(seed: int = 0) -> dict:
    key = jax.random.key(seed)
    ks = jax.random.split(key, 17)

    def nrm(k, shape, fan):
        return jax.random.normal(k, shape, jnp.float32) * fan ** -0.5

    def gain(k):
        return 1.0 + 0.05 * jax.random.normal(k, (DEPTH, D_MODEL), jnp.float32)

    cmp_in = NSA_CMP_LEN * HEAD_DIM
    return {
        "x": jax.random.normal(ks[0], (BATCH, SEQ, D_MODEL), jnp.float32),
        "w_in": nrm(ks[1], (DEPTH, D_MODEL, PROJ_WIDTH), D_MODEL),
        "w_out": nrm(ks[2], (DEPTH, MIX_WIDTH, D_MODEL), MIX_WIDTH),
        "g_mix_pre": gain(ks[3]),
        "g_mix_post": gain(ks[4]),
        "g_mlp_pre": gain(ks[5]),
        "g_mlp_post": gain(ks[6]),
        "w_up": nrm(ks[7], (DEPTH, D_MODEL, D_FF), D_MODEL),
        "w_down": nrm(ks[8], (DEPTH, D_FF, D_MODEL), D_FF),
        "cmp_pe": 0.1 * jax.random.normal(ks[9], (DEPTH, NSA_CMP_LEN, HEAD_DIM), jnp.float32),
        "phik_w1": nrm(ks[10], (DEPTH, cmp_in, NSA_CMP_HIDDEN), cmp_in),
        "phik_w2": nrm(ks[11], (DEPTH, NSA_CMP_HIDDEN, HEAD_DIM), NSA_CMP_HIDDEN),
        "phiv_w1": nrm(ks[12], (DEPTH, cmp_in, NSA_CMP_HIDDEN), cmp_in),
        "phiv_w2": nrm(ks[13], (DEPTH, NSA_CMP_HIDDEN, HEAD_DIM), NSA_CMP_HIDDEN),
        "fox_bias": 4.0 + 0.5 * jax.random.normal(ks[14], (DEPTH, HEADS_PER_MIXER), jnp.float32),
        "rel_bias": 0.5 * jax.random.normal(ks[15], (N_BUCKETS, N_BIAS_HEADS), jnp.float32),
    }


def reference(x, w_in, w_out, g_mix_pre, g_mix_post, g_mlp_pre, g_mlp_post, w_up, w_down,
              cmp_pe, phik_w1, phik_w2, phiv_w1, phiv_w2, fox_bias, rel_bias):
    for l in range(DEPTH):
        h = rmsnorm(x, g_mix_pre[l])
        mix = hybrid_mixer(h, w_in[l], w_out[l], cmp_pe[l], phik_w1[l], phik_w2[l],
                           phiv_w1[l], phiv_w2[l], fox_bias[l], rel_bias)
        x = x + rmsnorm(mix, g_mix_post[l])
        h2 = rmsnorm(x, g_mlp_pre[l])
        y = jnp.square(jax.nn.relu(h2 @ w_up[l])) @ w_down[l]
        x = x + rmsnorm(y, g_mlp_post[l])
    return x
```

```python
import math
import numpy as np
import ml_dtypes
from contextlib import ExitStack
import concourse.bass as bass
import concourse.mybir as mybir
from concourse.bass_utils import run_bass_kernel_spmd

F32 = mybir.dt.float32
BF16 = mybir.dt.bfloat16
U8 = mybir.dt.uint8
AF = mybir.ActivationFunctionType
ALU = mybir.AluOpType
AX = mybir.AxisListType


class Trk:
    __slots__ = ('w', 'r', 'ro')

    def __init__(self, ro=False):
        self.w = None
        self.r = {}
        self.ro = ro


class Sched:
    ENG = ['pe', 'act', 'dve', 'pool', 'sp']

    def __init__(self, nc, ndma=8):
        self.nc = nc
        self.ops = {e: [] for e in self.ENG}
        self.cnt = {e: 0 for e in self.ENG}
        self.seen = {e: {} for e in self.ENG}
        self.dma_i = {e: 0 for e in self.ENG}
        self.ndma = ndma
        self.semvals = {}
        self.es = ExitStack()
        self.arena = None
        self.arena_off = 0
        self.arena_size = 0

    def init_arena(self, nbytes):
        self.arena = self.es.enter_context(self.nc.sbuf_tensor("arena", [128, nbytes], U8))
        self.arena_size = nbytes
        self.arena_off = 0

    def sb(self, shape, dtype, parts=128):
        n = int(np.prod(shape))
        esz = 4 if dtype == F32 else (2 if dtype == BF16 else 1)
        nb = (n * esz + 31) // 32 * 32
        assert self.arena_off + nb <= self.arena_size, ("arena overflow", self.arena_off, nb)
        v = self.arena[:, self.arena_off:self.arena_off + n * esz]
        self.arena_off += nb
        if dtype != U8:
            v = v.bitcast(dtype)
        if len(shape) == 2:
            v = v.rearrange("p (a b) -> p a b", b=shape[1])
        elif len(shape) == 3:
            v = v.rearrange("p (a b c) -> p a b c", b=shape[1], c=shape[2])
        return v

    def psum(self, name, shape=(128, 512), dtype=F32):
        return self.es.enter_context(self.nc.psum_tensor(name, list(shape), dtype)).ap()

    def _waits(self, eng, reads, writes):
        need = {}

        def add(s, v):
            if s == 'pe' and eng == 'pe':
                return
            if self.seen[eng].get(s, 0) >= v:
                return
            if need.get(s, 0) < v:
                need[s] = v
        for t in reads:
            if t.w is not None:
                add(*t.w)
        for t in writes:
            if t.w is not None:
                add(*t.w)
            for s, v in t.r.items():
                add(s, v)
        for s, v in need.items():
            self.seen[eng][s] = v
            self.ops[eng].append(('w', s, v))

    def _commit(self, ev, reads, writes):
        s, v = ev
        for t in reads:
            if not t.ro:
                if t.r.get(s, 0) < v:
                    t.r[s] = v
        for t in writes:
            t.w = ev
            t.r = {}
        if self.semvals.get(s, 0) < v:
            self.semvals[s] = v

    def op(self, eng, fn, reads=(), writes=()):
        self._waits(eng, reads, writes)
        self.cnt[eng] += 1
        ev = (eng, self.cnt[eng])
        self.ops[eng].append(('i', fn, eng, 1))
        self._commit(ev, reads, writes)
        return ev

    def dma(self, q, out, in_, reads=(), writes=(), **kw):
        i = self.dma_i[q]
        self.dma_i[q] += 1
        sname = 'd_%s_%d' % (q, i % self.ndma)
        prev = 16 * (i // self.ndma)
        if prev > 0 and self.seen[q].get(sname, 0) < prev:
            self.seen[q][sname] = prev
            self.ops[q].append(('w', sname, prev))
        self._waits(q, reads, writes)
        ev = (sname, prev + 16)
        self.ops[q].append(('i', (lambda e, out=out, in_=in_, kw=kw: e.dma_start(out=out, in_=in_, **kw)), sname, 16))
        self._commit(ev, reads, writes)
        return ev

    def barrier(self):
        for e in self.ENG:
            for s, v in self.semvals.items():
                if s == e and e == 'pe':
                    continue
                if self.seen[e].get(s, 0) < v:
                    self.seen[e][s] = v
                    self.ops[e].append(('w', s, v))

    def emit(self):
        nc = self.nc
        self.barrier()
        sems = {}
        for s in self.semvals:
            sems[s] = self.es.enter_context(nc.semaphore(s))
        engmap = {'pe': 'tensor', 'act': 'scalar', 'dve': 'vector', 'pool': 'gpsimd', 'sp': 'sync'}
        with nc.Block() as block:
            for e in self.ENG:
                ops = self.ops[e]

                def body(eng, ops=ops):
                    for o in ops:
                        if o[0] == 'w':
                            eng.wait_ge(sems[o[1]], o[2])
                        else:
                            ins = o[1](eng)
                            ins.then_inc(sems[o[2]], o[3])
                getattr(block, engmap[e])(body)
        self.es.close()

BFNP = ml_dtypes.bfloat16
NEGM = -30000.0
BIGV = 1e30
WT_, WW_, WC_, WD_ = 3968, 1408, 5632, 2944


def mm(out, l, r, st, sp):
    return lambda e: e.matmul(out, lhsT=l, rhs=r, start=st, stop=sp)


class Ctx:
    pass


def _mark(S):
    return S.arena_off


def _reset(S, off):
    S.arena_off = off


def rstd_from_ps(S, ps_ap, out_ap, t_ps, t_out, epscol, t_c, scale):
    S.op('act', lambda e: e.activation(out=out_ap, in_=ps_ap, func=AF.Ln, bias=epscol, scale=scale),
         reads=[t_ps, t_c], writes=[t_out])
    S.op('act', lambda e: e.activation(out=out_ap, in_=out_ap, func=AF.Exp, scale=-0.5),
         reads=[t_out], writes=[t_out])


def attn_tiles(S, C, qrhs, t_q, tiles, KQ=64):
    n = len(tiles)
    for i, tl in enumerate(tiles):
        b = C.tctr % 2
        C.tctr += 1
        st, t_st = C.st[b], C.t_st[b]
        pe_, t_pe = C.pebuf[b], C.t_pebuf[b]
        pt_, t_pt = C.ptbuf[b], C.t_ptbuf[b]
        ex = tl.get('extra')
        S.op('pe', mm(st, tl['kT'], qrhs, True, ex is None), reads=[tl['t_k'], t_q], writes=[t_st])
        if ex is not None:
            S.op('pe', mm(st, ex[0], ex[1], False, True), reads=[ex[2], C.t_const], writes=[t_st])
        bias = tl.get('bias')
        if bias is not None:
            S.op('act', lambda e, pe_=pe_, st=st, bias=bias: e.activation(out=pe_, in_=st, func=AF.Exp, bias=bias, scale=1.0),
                 reads=[t_st, tl['t_bias']], writes=[t_pe])
        else:
            S.op('act', lambda e, pe_=pe_, st=st: e.activation(out=pe_, in_=st, func=AF.Exp), reads=[t_st], writes=[t_pe])
        band = tl.get('band')
        if band is not None:
            S.op('dve', lambda e, pt_=pt_, pe_=pe_, band=band: e.tensor_tensor(out=pt_, in0=pe_, in1=band, op=ALU.mult),
                 reads=[t_pe, tl['t_band']], writes=[t_pt])
            P, t_P = pt_, t_pt
        else:
            P, t_P = pe_, t_pe
        keep = tl.get('keep')
        if keep is not None:
            S.op('pool', lambda e, k=keep[0], P=P: e.tensor_copy(out=k, in_=P), reads=[t_P], writes=[keep[1]])
        S.op('pe', mm(C.num[0:64, :], tl['v'], P, i == 0, i == n - 1), reads=[tl['t_v'], t_P], writes=[C.t_num])
        S.op('pe', mm(C.den[0:64, :], C.ones_bf[:, 0:64], P, i == 0, i == n - 1), reads=[C.t_const, t_P], writes=[C.t_den])


def attn_finish(S, C, out_ap, t_out):
    S.op('dve', lambda e: e.tensor_scalar(out=C.rd[0:64], in0=C.den[0:64, :], scalar1=1e-30, scalar2=None, op0=ALU.max), reads=[C.t_den], writes=[C.t_rd])
    S.op('dve', lambda e: e.reciprocal(out=C.rd[0:64], in_=C.rd[0:64]), reads=[C.t_rd], writes=[C.t_rd])
    S.op('dve', lambda e, o=out_ap: e.tensor_tensor(out=o, in0=C.num[0:64, :], in1=C.rd[0:64], op=ALU.mult),
         reads=[C.t_num, C.t_rd], writes=[t_out])


def build_A(T):
    NS, NB, NKB = T // 512, T // 128, T // 256
    NSW = T // 64
    NCB = max(1, T // 2048)
    NCMP = T // 16 - 1
    nc = bass.Bass("TRN2", target_bir_lowering=False)

    def din(n, sh, dt=F32):
        return nc.dram_tensor(n, list(sh), dt, kind="ExternalInput").ap()

    def dsc(n, sh, dt=F32):
        return nc.dram_tensor(n, list(sh), dt, kind="Internal").ap()
    xT = din("xT", [2048, T]); w = din("w", [2048, 2960]); gpre = din("gpre", [128, 16]); foxb = din("foxb", [128, 4])
    zta = din("zta", [4, 128, WT_]); ztb = din("ztb", [4, 128, WT_]); zw = din("zw", [4, 128, WW_])
    zc = din("zc", [4, 128, WC_]); zd = din("zd", [4, 128, WD_])
    cd = din("cd", [128, WD_], BF16); ccm = din("ccm", [128, 896], BF16)
    ident = din("ident", [128, 128], BF16); identf = din("identf", [128, 128])
    ohA = din("ohA", [128, NB * 128], BF16); ohB = din("ohB", [32, 32 * 128], BF16)
    ovl = din("ovl", [128, NCB, NSW + 1], BF16)
    vmb = din("vmb", [128, 2 * NSW]); amb = din("amb", [128, 2 * NSW])
    tri = din("tri", [128, 128]); r0m = din("r0m", [128, 128])
    w1k = din("w1k", [2048, 256]); w2k = din("w2k", [256, 64]); w1v = din("w1v", [2048, 256]); w2v = din("w2v", [256, 64])
    peT = din("peT", [64, 32])
    oT = nc.dram_tensor("oT", [1024, T], BF16, kind="ExternalOutput").ap()
    FMd = dsc("FMd", [2048, T], BF16); GAd = dsc("GAd", [12, T]); GSd = dsc("GSd", [12, T])
    TMd = dsc("TMd", [T, 896], BF16); FCd = dsc("FCd", [T, 4])
    OCd = dsc("OCd", [256, T]); NMd = dsc("NMd", [NS, 128, 512], BF16)

    S = Sched(nc)
    S.init_arena(200 * 1024)
    banks = [S.psum("pb%d" % i) for i in range(7)]
    t_bank = [Trk() for _ in range(7)]
    pbb = S.psum("pbb", (128, 1024), BF16)
    t_pbb = Trk()
    out_evs = []

    C = Ctx()
    C.t_const = Trk()
    C.ones_bf = S.sb([128], BF16)
    onesf = S.sb([128], F32)
    epscol = S.sb([1], F32)
    onecol = S.sb([1], F32)
    S.op('pool', lambda e: e.memset(C.ones_bf, 1.0), writes=[C.t_const])
    S.op('pool', lambda e: e.memset(onesf, 1.0), writes=[C.t_const])
    S.op('pool', lambda e: e.memset(epscol, 1e-6), writes=[C.t_const])
    S.op('pool', lambda e: e.memset(onecol, 1.0), writes=[C.t_const])
    gp = S.sb([16], F32)
    S.dma('sp', gp, gpre, writes=[C.t_const])
    base0 = _mark(S)

    Wb = S.sb([16, 2960], BF16); t_W = Trk()
    m1 = _mark(S)
    wst = [S.sb([2960], F32) for _ in range(2)]; t_wst = [Trk(), Trk()]
    for kc in range(16):
        sl = kc % 2
        S.dma('sp', wst[sl], w[kc * 128:(kc + 1) * 128, :], writes=[t_wst[sl]])
        eng = 'dve' if kc % 2 == 0 else 'pool'
        S.op(eng, lambda e, kc=kc, sl=sl: e.tensor_copy(out=Wb[:, kc, :], in_=wst[sl]), reads=[t_wst[sl]], writes=[t_W])
    S.barrier()
    _reset(S, m1)
    xs = [S.sb([4, 512], F32) for _ in range(2)]; t_xs = [Trk(), Trk()]
    hb = [S.sb([16, 512], BF16) for _ in range(2)]; t_hb = [Trk(), Trk()]
    sq = [S.sb([16, 512], BF16) for _ in range(2)]; t_sq = [Trk(), Trk()]
    rb = [S.sb([512], F32) for _ in range(2)]; t_rb = [Trk(), Trk()]
    rt = [S.sb([4], F32) for _ in range(2)]; t_rt = [Trk(), Trk()]
    ob = [S.sb([512], BF16) for _ in range(4)]; t_ob = [Trk() for _ in range(4)]
    obf = S.sb([512], F32); t_obf = Trk()
    tmo = [S.sb([450], BF16) for _ in range(2)]; t_tmo = [Trk(), Trk()]
    fco = [S.sb([4], F32) for _ in range(2)]; t_fco = [Trk(), Trk()]
    QCH = (0, 1, 4, 5, 8, 9, 12, 13)
    xi = 0
    for s in range(NS):
        sl = s % 2
        cs = slice(s * 512, (s + 1) * 512)
        for kg in range(4):
            xl = xi % 2; xi += 1
            S.dma('sp', xs[xl], xT[kg * 512:(kg + 1) * 512, cs].rearrange("(c p) t -> p c t", p=128), writes=[t_xs[xl]])
            for c4 in range(4):
                kc = kg * 4 + c4
                S.op('dve', lambda e, kc=kc, c4=c4, xl=xl, sl=sl: e.tensor_scalar(out=hb[sl][:, kc, :], in0=xs[xl][:, c4, :], scalar1=gp[:, kc:kc + 1], scalar2=None, op0=ALU.mult),
                     reads=[t_xs[xl], C.t_const], writes=[t_hb[sl]])
            S.op('act', lambda e, kg=kg, xl=xl, sl=sl: e.activation(out=sq[sl][:, kg * 4:(kg + 1) * 4, :], in_=xs[xl], func=AF.Square),
                 reads=[t_xs[xl]], writes=[t_sq[sl]])
        for kc in range(16):
            S.op('pe', mm(banks[4], C.ones_bf, sq[sl][:, kc, :], kc == 0, kc == 15), reads=[t_sq[sl], C.t_const], writes=[t_bank[4]])
        rstd_from_ps(S, banks[4], rb[sl], t_bank[4], t_rb[sl], epscol, C.t_const, 1.0 / 2048)
        for tb in range(4):
            for kc in range(16):
                S.op('pe', mm(banks[5][:, tb:tb + 1], sq[sl][:, kc, tb * 128:(tb + 1) * 128], C.ones_bf[:, 0:1], kc == 0, kc == 15),
                     reads=[t_sq[sl], C.t_const], writes=[t_bank[5]])
        rstd_from_ps(S, banks[5][:, 0:4], rt[sl], t_bank[5], t_rt[sl], epscol, C.t_const, 1.0 / 2048)
        for cc in range(17):
            M = 128 if cc < 16 else 12
            bk = cc % 4
            for kc in range(16):
                S.op('pe', mm(banks[bk][0:M, :], Wb[:, kc, cc * 128:cc * 128 + M], hb[sl][:, kc, :], kc == 0, kc == 15),
                     reads=[t_W, t_hb[sl]], writes=[t_bank[bk]])
            if cc < 16:
                o_, t_o = ob[cc % 4], t_ob[cc % 4]
                if cc in QCH:
                    S.op('dve', lambda e, o_=o_, bk=bk, sl=sl: e.scalar_tensor_tensor(out=o_, in0=banks[bk], scalar=0.125, in1=rb[sl], op0=ALU.mult, op1=ALU.mult),
                         reads=[t_bank[bk], t_rb[sl]], writes=[t_o])
                else:
                    S.op('dve', lambda e, o_=o_, bk=bk, sl=sl: e.tensor_tensor(out=o_, in0=banks[bk], in1=rb[sl], op=ALU.mult),
                         reads=[t_bank[bk], t_rb[sl]], writes=[t_o])
                S.dma('pool', FMd[cc * 128:(cc + 1) * 128, cs], o_, reads=[t_o])
            else:
                S.op('dve', lambda e, bk=bk, sl=sl: e.tensor_tensor(out=obf[0:12], in0=banks[bk][0:12, :], in1=rb[sl][0:12], op=ALU.mult),
                     reads=[t_bank[bk], t_rb[sl]], writes=[t_obf])
                S.dma('pool', GAd[:, cs], obf[0:12], reads=[t_obf])
        for tb in range(4):
            for cg in range(2):
                bk = 5 + ((tb * 2 + cg) % 2)
                if bk == 5:
                    bk = 4
                for kc in range(16):
                    S.op('pe', mm(banks[bk][:, 0:450], hb[sl][:, kc, tb * 128:(tb + 1) * 128], Wb[:, kc, 2060 + cg * 450:2060 + (cg + 1) * 450], kc == 0, kc == 15),
                         reads=[t_W, t_hb[sl]], writes=[t_bank[bk]])
                tl = (tb * 2 + cg) % 2
                rows = slice(s * 512 + tb * 128, s * 512 + (tb + 1) * 128)
                if cg == 0:
                    S.op('act', lambda e, bk=bk, tl=tl, tb=tb, sl=sl: e.activation(out=tmo[tl], in_=banks[bk][:, 0:450], func=AF.Copy, scale=rt[sl][:, tb:tb + 1]),
                         reads=[t_bank[bk], t_rt[sl]], writes=[t_tmo[tl]])
                    S.dma('pool', TMd[rows, 0:450], tmo[tl], reads=[t_tmo[tl]])
                else:
                    S.op('act', lambda e, bk=bk, tl=tl, tb=tb, sl=sl: e.activation(out=tmo[tl][:, 0:446], in_=banks[bk][:, 0:446], func=AF.Copy, scale=rt[sl][:, tb:tb + 1]),
                         reads=[t_bank[bk], t_rt[sl]], writes=[t_tmo[tl]])
                    S.op('act', lambda e, bk=bk, tl=tl, tb=tb, sl=sl: e.activation(out=fco[tb % 2], in_=banks[bk][:, 446:450], func=AF.Copy, scale=rt[sl][:, tb:tb + 1]),
                         reads=[t_bank[bk], t_rt[sl]], writes=[t_fco[tb % 2]])
                    S.dma('pool', TMd[rows, 450:896], tmo[tl][:, 0:446], reads=[t_tmo[tl]])
                    S.dma('pool', FCd[rows, :], fco[tb % 2], reads=[t_fco[tb % 2]])
    S.barrier()
    _reset(S, base0)

    identb = S.sb([128], BF16); identfs = S.sb([128], F32)
    S.dma('sp', identb, ident, writes=[C.t_const])
    S.dma('sp', identfs, identf, writes=[C.t_const])
    C.st = [banks[0], banks[1]]; C.t_st = [t_bank[0], t_bank[1]]
    C.num, C.t_num = banks[2], t_bank[2]
    C.den, C.t_den = banks[3], t_bank[3]
    C.pebuf = [S.sb([512], BF16) for _ in range(2)]; C.t_pebuf = [Trk(), Trk()]
    C.ptbuf = [S.sb([512], BF16) for _ in range(2)]; C.t_ptbuf = [Trk(), Trk()]
    C.rd = S.sb([512], F32); C.t_rd = Trk()
    C.tctr = 0
    zst = S.sb([WC_], F32); t_zst = Trk()
    base1 = _mark(S)

    def load_band(dst, t_dst, src, W, mult=None):
        S.dma('sp', zst[:, 0:W], src, writes=[t_zst])
        S.op('act', lambda e: e.activation(out=dst[:, 0:W], in_=zst[:, 0:W], func=AF.Exp), reads=[t_zst], writes=[t_dst])
        if mult is not None:
            S.op('dve', lambda e: e.tensor_tensor(out=dst[:, 0:W], in0=dst[:, 0:W], in1=mult[0], op=ALU.mult), reads=[t_dst, mult[1]], writes=[t_dst])

    def load_v(dst, t_dst, col):
        S.dma('sp', dst, TMd[:, col:col + 64].rearrange("(j p) d -> p j d", p=128), writes=[t_dst])

    w1b = S.sb([32, 256], BF16); t_w1 = Trk()
    w2kb = S.sb([2, 64], BF16); w2vb = S.sb([2, 64], BF16); t_w2 = Trk()
    pebf = S.sb([32], BF16); t_pe32 = Trk()
    kxT = S.sb([T], BF16); t_kx = Trk()
    hm = S.sb([2, 512 * NCB], BF16); t_hm = Trk()
    kcT = S.sb([128 * NCB], BF16); t_kcT = Trk()
    vcp = S.sb([NCB, 64], BF16); t_vcp = Trk()
    pbias = S.sb([2], F32); t_pbias = Trk()
    g1 = S.sb([512], F32); g2 = S.sb([512], F32); t_g1 = Trk(); t_g2 = Trk()
    m2 = _mark(S)
    w1st = S.sb([32, 256], F32); t_w1st = Trk()
    w2st = S.sb([2, 64], F32); t_w2st = Trk()
    pest = S.sb([32], F32)
    S.dma('sp', pest[0:64], peT, writes=[t_pe32])
    S.op('dve', lambda e: e.tensor_copy(out=pebf[0:64], in_=pest[0:64]), reads=[t_pe32], writes=[t_pe32])
    NCOL = NCMP
    for which in range(2):
        w1src = w1k if which == 0 else w1v
        w2src = w2k if which == 0 else w2v
        w2b = w2kb if which == 0 else w2vb
        S.dma('sp', w1st[0:64], w1src.rearrange("(i d) h -> d i h", d=64), writes=[t_w1st])
        S.op('dve', lambda e: e.tensor_copy(out=w1b[0:64], in_=w1st[0:64]), reads=[t_w1st], writes=[t_w1])
        S.dma('sp', w2st, w2src.rearrange("(c p) d -> p c d", p=128), writes=[t_w2st])
        S.op('dve', lambda e, w2b=w2b: e.tensor_copy(out=w2b, in_=w2st), reads=[t_w2st], writes=[t_w2])
        S.dma('sp', kxT[0:64], FMd[256 + which * 64:320 + which * 64, :], writes=[t_kx])
        S.op('pool', lambda e: e.memset(hm, 0.0), writes=[t_hm])
        for hc in range(2):
            for i in range(32):
                S.op('pe', mm(banks[4][:, 0:1], w1b[0:64, i, hc * 128:(hc + 1) * 128], pebf[0:64, i:i + 1], i == 0, i == 31),
                     reads=[t_w1, t_pe32], writes=[t_bank[4]])
            S.op('dve', lambda e, hc=hc: e.tensor_copy(out=pbias[:, hc:hc + 1], in_=banks[4][:, 0:1]), reads=[t_bank[4]], writes=[t_pbias])
            c0 = 0
            while c0 < NCOL:
                N = min(512, NCOL - c0)
                for i in range(32):
                    S.op('pe', mm(banks[5][:, 0:N], w1b[0:64, i, hc * 128:(hc + 1) * 128], kxT[0:64, i + 16 * c0:i + 16 * (c0 + N - 1) + 1:16], i == 0, i == 31),
                         reads=[t_w1, t_kx], writes=[t_bank[5]])
                S.op('act', lambda e, hc=hc, N=N: e.activation(out=g1[:, 0:N], in_=banks[5][:, 0:N], func=AF.Identity, bias=pbias[:, hc:hc + 1], scale=1.0),
                     reads=[t_bank[5], t_pbias], writes=[t_g1])
                S.op('dve', lambda e, N=N: e.tensor_tensor(out=g2[:, 0:N], in0=g1[:, 0:N], in1=g1[:, 0:N], op=ALU.mult), reads=[t_g1], writes=[t_g2])
                S.op('dve', lambda e, N=N: e.tensor_scalar(out=g2[:, 0:N], in0=g2[:, 0:N], scalar1=0.044715, scalar2=1.0, op0=ALU.mult, op1=ALU.add), reads=[t_g2], writes=[t_g2])
                S.op('dve', lambda e, N=N: e.tensor_tensor(out=g2[:, 0:N], in0=g2[:, 0:N], in1=g1[:, 0:N], op=ALU.mult), reads=[t_g1, t_g2], writes=[t_g2])
                S.op('act', lambda e, N=N: e.activation(out=g2[:, 0:N], in_=g2[:, 0:N], func=AF.Sigmoid, scale=2.0 * math.sqrt(2.0 / math.pi)), reads=[t_g2], writes=[t_g2])
                S.op('dve', lambda e, hc=hc, N=N, c0=c0: e.tensor_tensor(out=hm[:, hc, c0:c0 + N], in0=g2[:, 0:N], in1=g1[:, 0:N], op=ALU.mult), reads=[t_g1, t_g2], writes=[t_hm])
                c0 += N
        if which == 0:
            S.op('pool', lambda e: e.memset(kcT, 0.0), writes=[t_kcT])
            c0 = 0
            while c0 < NCOL:
                N = min(512, NCOL - c0)
                for hc in range(2):
                    S.op('pe', mm(banks[4][0:64, 0:N], w2kb[:, hc, :], hm[:, hc, c0:c0 + N], hc == 0, hc == 1), reads=[t_w2, t_hm], writes=[t_bank[4]])
                S.op('dve', lambda e, c0=c0, N=N: e.tensor_copy(out=kcT[0:64, c0:c0 + N], in_=banks[4][0:64, 0:N]), reads=[t_bank[4]], writes=[t_kcT])
                c0 += N
        else:
            for cb in range(NCB):
                for hc in range(2):
                    S.op('pe', mm(banks[4][:, 0:64], hm[:, hc, cb * 128:(cb + 1) * 128], w2vb[:, hc, :], hc == 0, hc == 1), reads=[t_w2, t_hm], writes=[t_bank[4]])
                S.op('dve', lambda e, cb=cb: e.tensor_copy(out=vcp[:, cb, :], in_=banks[4][:, 0:64]), reads=[t_bank[4]], writes=[t_vcp])
    S.barrier()
    _reset(S, m2)
    ohAs = S.sb([NB * 128], BF16)
    S.dma('sp', ohAs, ohA, writes=[C.t_const])
    ovls = S.sb([NCB, NSW + 1], BF16)
    S.dma('sp', ovls, ovl, writes=[C.t_const])
    vms = S.sb([2 * NSW], F32); ams = S.sb([2 * NSW], F32)
    S.dma('sp', vms, vmb, writes=[C.t_const]); S.dma('sp', ams, amb, writes=[C.t_const])
    bandc = [S.sb([WC_], BF16) for _ in range(4)]; t_bandc = [Trk() for _ in range(4)]
    for r in range(4):
        load_band(bandc[r], t_bandc[r], zc[r], WC_)
    q4 = [S.sb([4, 512], BF16) for _ in range(2)]; t_q4 = [Trk(), Trk()]
    ptc = [S.sb([512], BF16) for _ in range(NCB)]; t_ptc = [Trk() for _ in range(NCB)]
    impacc = [S.sb([NSW], F32) for _ in range(4)]; t_imp = [Trk() for _ in range(4)]
    impf = S.sb([NSW], F32); t_impf = Trk()
    imp2 = S.sb([NSW], F32); t_imp2 = Trk()
    m8a = S.sb([8], F32); m8b = S.sb([8], F32); t_m8 = Trk()
    rd1 = S.sb([1], F32); t_rd1 = Trk()
    nmq = S.sb([NSW], BF16); t_nmq = Trk()
    nmT = [S.sb([512], BF16) for _ in range(2)]; t_nmT = [Trk(), Trk()]
    ocs = [S.sb([512], F32) for _ in range(2)]; t_ocs = [Trk(), Trk()]
    oci = 0
    for s in range(NS):
        sl = s % 2
        cs = slice(s * 512, (s + 1) * 512)
        S.dma('sp', q4[sl][0:64], FMd[0:256, cs].rearrange("(r d) t -> d r t", d=64), writes=[t_q4[sl]])
        for r in range(4):
            tiles = []
            for jc in range(NCB):
                off = 512 * s - 2048 * jc
                if off < 0:
                    continue
                off = min(off, 5120)
                tiles.append(dict(kT=kcT[0:64, jc * 128:(jc + 1) * 128], t_k=t_kcT, v=vcp[:, jc, :], t_v=t_vcp,
                                  band=bandc[r][:, off:off + 512], t_band=t_bandc[r], keep=(ptc[jc], t_ptc[jc]), jc=jc))
            attn_tiles(S, C, q4[sl][0:64, r, :], t_q4[sl], tiles)
            for qb in range(4):
                for ti, tl in enumerate(tiles):
                    jc = tl['jc']
                    S.op('pe', mm(banks[4][:, 0:NSW + 1], ptc[jc][:, qb * 128:(qb + 1) * 128], ovls[:, jc, :], ti == 0, ti == len(tiles) - 1),
                         reads=[t_ptc[jc], C.t_const], writes=[t_bank[4]])
                S.op('dve', lambda e: e.tensor_scalar(out=rd1, in0=banks[4][:, NSW:NSW + 1], scalar1=1e-30, scalar2=None, op0=ALU.max), reads=[t_bank[4]], writes=[t_rd1])
                S.op('dve', lambda e: e.reciprocal(out=rd1, in_=rd1), reads=[t_rd1], writes=[t_rd1])
                if r == 0:
                    S.op('dve', lambda e, qb=qb: e.tensor_scalar(out=impacc[qb], in0=banks[4][:, 0:NSW], scalar1=rd1, scalar2=None, op0=ALU.mult),
                         reads=[t_bank[4], t_rd1], writes=[t_imp[qb]])
                else:
                    S.op('dve', lambda e, qb=qb: e.scalar_tensor_tensor(out=impacc[qb], in0=banks[4][:, 0:NSW], scalar=rd1, in1=impacc[qb], op0=ALU.mult, op1=ALU.add),
                         reads=[t_bank[4], t_rd1, t_imp[qb]], writes=[t_imp[qb]])
            ol = oci % 2; oci += 1
            attn_finish(S, C, ocs[ol][0:64], t_ocs[ol])
            S.dma('pool', OCd[r * 64:(r + 1) * 64, cs], ocs[ol][0:64], reads=[t_ocs[ol]])
        for qb in range(4):
            i = 4 * s + qb
            st0 = (NSW - 2) - 2 * i
            S.op('dve', lambda e, qb=qb, st0=st0: e.tensor_tensor(out=impf, in0=impacc[qb], in1=vms[:, st0:st0 + NSW], op=ALU.mult), reads=[t_imp[qb], C.t_const], writes=[t_impf])
            S.op('dve', lambda e, st0=st0: e.tensor_tensor(out=impf, in0=impf, in1=ams[:, st0:st0 + NSW], op=ALU.add), reads=[t_impf, C.t_const], writes=[t_impf])
            S.op('dve', lambda e: e.memset(impf[:, 0:1], BIGV), reads=[t_impf], writes=[t_impf])
            S.op('dve', lambda e: e.max(out=m8a, in_=impf), reads=[t_impf], writes=[t_m8])
            S.op('dve', lambda e: e.match_replace(out=imp2, in_to_replace=m8a, in_values=impf, imm_value=-BIGV), reads=[t_impf, t_m8], writes=[t_imp2])
            S.op('dve', lambda e: e.max(out=m8b, in_=imp2), reads=[t_imp2], writes=[t_m8])
            S.op('dve', lambda e: e.tensor_scalar(out=nmq, in0=impf, scalar1=m8b[:, 7:8], scalar2=NEGM, op0=ALU.is_lt, op1=ALU.mult), reads=[t_impf, t_m8], writes=[t_nmq])
            S.op('pe', lambda e, qb=qb: e.transpose(out=pbb[0:NSW, qb * 128:(qb + 1) * 128], in_=nmq, identity=identb), reads=[t_nmq, C.t_const], writes=[t_pbb])
        S.op('dve', lambda e, sl=sl: e.tensor_copy(out=nmT[sl][0:NSW], in_=pbb[0:NSW, 0:512]), reads=[t_pbb], writes=[t_nmT[sl]])
        S.dma('pool', NMd[s, 0:NSW, :], nmT[sl][0:NSW], reads=[t_nmT[sl]])
    gsb = S.sb([T], F32); t_gsb = Trk()
    S.dma('sp', gsb[0:12], GAd, writes=[t_gsb])
    S.op('act', lambda e: e.activation(out=gsb[0:12], in_=gsb[0:12], func=AF.Sigmoid), reads=[t_gsb], writes=[t_gsb])
    S.dma('pool', GSd, gsb[0:12], reads=[t_gsb])
    S.barrier()
    _reset(S, base1)

    ohAs = S.sb([NB * 128], BF16)
    S.dma('sp', ohAs, ohA, writes=[C.t_const])
    ksT = S.sb([T], BF16); kwT = S.sb([T], BF16); t_ks = Trk(); t_kw = Trk()
    vs = S.sb([NB, 64], BF16); vw = S.sb([NB, 64], BF16); t_vs = Trk(); t_vw = Trk()
    S.dma('sp', ksT[0:64], FMd[384:448, :], writes=[t_ks])
    S.dma('sp', kwT[0:64], FMd[448:512, :], writes=[t_kw])
    load_v(vs, t_vs, 0); load_v(vw, t_vw, 64)
    qT = S.sb([T], BF16); t_qT = Trk()
    bandt = S.sb([WT_], BF16); t_bandt = Trk()
    bandw = S.sb([WW_], BF16); t_bandw = Trk()
    nmTs = [S.sb([512], BF16) for _ in range(2)]; t_nmTs = [Trk(), Trk()]
    o_s = S.sb([512], F32); o_w = S.sb([512], F32); o_c = S.sb([512], F32); t_os = Trk(); t_ow = Trk(); t_oc = Trk()
    gbs = [S.sb([512], F32) for _ in range(3)]; t_gbs = [Trk() for _ in range(3)]
    oo = [S.sb([512], BF16) for _ in range(2)]; t_oo = [Trk(), Trk()]
    ni = 0
    for r in range(4):
        S.dma('sp', qT[0:64], FMd[r * 64:(r + 1) * 64, :], writes=[t_qT])
        load_band(bandt, t_bandt, zta[r], WT_)
        load_band(bandw, t_bandw, zw[r], WW_)
        for s in range(NS):
            cs = slice(s * 512, (s + 1) * 512)
            nl = ni % 2; ni += 1
            S.dma('sp', nmTs[nl][0:NSW], NMd[s, 0:NSW, :], writes=[t_nmTs[nl]])
            for b3 in range(3):
                row = b3 * 4 + r
                S.dma('sp', gbs[b3][0:64], bass.AP(GSd.tensor, row * T + s * 512, [[0, 64], [1, 512]]), writes=[t_gbs[b3]])
            S.dma('sp', o_c[0:64], OCd[r * 64:(r + 1) * 64, cs], writes=[t_oc])
            tiles = []
            for j in range(4 * s + 4):
                off = min(512 * s - 128 * j, 3072) + 384
                tiles.append(dict(kT=ksT[0:64, j * 128:(j + 1) * 128], t_k=t_ks, v=vs[:, j, :], t_v=t_vs,
                                  extra=(ohAs[0:NSW, j * 128:(j + 1) * 128], nmTs[nl][0:NSW], t_nmTs[nl]),
                                  band=bandt[:, off:off + 512], t_band=t_bandt))
            attn_tiles(S, C, qT[0:64, cs], t_qT, tiles)
            attn_finish(S, C, o_s[0:64], t_os)
            tiles = []
            for j in range(max(0, 4 * s - 4), 4 * s + 4):
                off = (512 * s - 128 * j) + 384
                tiles.append(dict(kT=kwT[0:64, j * 128:(j + 1) * 128], t_k=t_kw, v=vw[:, j, :], t_v=t_vw,
                                  band=bandw[:, off:off + 512], t_band=t_bandw))
            attn_tiles(S, C, qT[0:64, cs], t_qT, tiles)
            attn_finish(S, C, o_w[0:64], t_ow)
            ol = (r * NS + s) % 2
            S.op('pool', lambda e: e.tensor_tensor(out=o_c[0:64], in0=o_c[0:64], in1=gbs[0][0:64], op=ALU.mult), reads=[t_oc, t_gbs[0]], writes=[t_oc])
            S.op('pool', lambda e: e.tensor_tensor(out=o_s[0:64], in0=o_s[0:64], in1=gbs[1][0:64], op=ALU.mult), reads=[t_os, t_gbs[1]], writes=[t_os])
            S.op('dve', lambda e: e.tensor_tensor(out=o_w[0:64], in0=o_w[0:64], in1=gbs[2][0:64], op=ALU.mult), reads=[t_ow, t_gbs[2]], writes=[t_ow])
            S.op('pool', lambda e: e.tensor_tensor(out=o_c[0:64], in0=o_c[0:64], in1=o_s[0:64], op=ALU.add), reads=[t_oc, t_os], writes=[t_oc])
            S.op('dve', lambda e, ol=ol: e.tensor_tensor(out=oo[ol][0:64], in0=o_c[0:64], in1=o_w[0:64], op=ALU.add), reads=[t_oc, t_ow], writes=[t_oo[ol]])
            out_evs.append(S.dma('pool', oT[r * 64:(r + 1) * 64, cs], oo[ol][0:64], reads=[t_oo[ol]]))
    S.barrier()
    _reset(S, base1)

    qT = S.sb([T], BF16); t_qT = Trk()
    kT = S.sb([T], BF16); t_kT = Trk()
    vv = S.sb([NB, 64], BF16); t_vv = Trk()
    bandt = S.sb([WT_], BF16); t_bandt = Trk()
    ohBs = S.sb([32 * 128], BF16)
    S.dma('sp', ohBs[0:32], ohB, writes=[C.t_const])
    cds = S.sb([WD_], BF16); t_cds = Trk()
    S.dma('sp', cds, cd, writes=[t_cds])
    ccs = S.sb([896], BF16); t_ccs = Trk()
    S.dma('sp', ccs, ccm, writes=[t_ccs])
    GW = max(NKB, 8)
    kmf = S.sb([NKB], F32); kmT = S.sb([NKB], BF16); t_km = Trk()
    gm = S.sb([GW], F32); t_gm = Trk()
    m8B = S.sb([8], F32); t_m8 = Trk()
    nmqB = S.sb([GW], BF16); t_nmq = Trk()
    nmTB = [S.sb([512], BF16) for _ in range(2)]; t_nmT = [Trk(), Trk()]
    ooB = [S.sb([512], BF16) for _ in range(2)]; t_oo = [Trk(), Trk()]
    oi = 0
    for h in range(4):
        S.dma('sp', qT[0:64], FMd[512 + h * 64:512 + (h + 1) * 64, :], writes=[t_qT])
        S.dma('sp', kT[0:64], FMd[768 + h * 64:768 + (h + 1) * 64, :], writes=[t_kT])
        load_v(vv, t_vv, 128 + h * 64)
        load_band(bandt, t_bandt, ztb[h], WT_)
        S.op('dve', lambda e: e.tensor_reduce(out=kmf[0:64], in_=kT[0:64].rearrange("p (n k) -> p n k", k=256), axis=AX.X, op=ALU.add), reads=[t_kT], writes=[t_km])
        S.op('dve', lambda e: e.tensor_scalar(out=kmT[0:64], in0=kmf[0:64], scalar1=1.0 / 256, scalar2=None, op0=ALU.mult), reads=[t_km], writes=[t_km])
        for s in range(NS):
            cs = slice(s * 512, (s + 1) * 512)
            nl = (h * NS + s) % 2
            for qb in range(4):
                i = 4 * s + qb
                cur = i // 2
                S.op('pe', mm(banks[4][:, 0:NKB], qT[0:64, i * 128:(i + 1) * 128], kmT[0:64, 0:NKB], True, True), reads=[t_qT, t_km], writes=[t_bank[4]])
                S.op('pool', lambda e: e.memset(gm, -BIGV), writes=[t_gm])
                if cur > 0:
                    S.op('dve', lambda e, cur=cur: e.tensor_copy(out=gm[:, 0:cur], in_=banks[4][:, 0:cur]), reads=[t_bank[4], t_gm], writes=[t_gm])
                S.op('dve', lambda e: e.max(out=m8B, in_=gm), reads=[t_gm], writes=[t_m8])
                S.op('dve', lambda e, cur=cur: e.memset(gm[:, cur:cur + 1], BIGV), reads=[t_gm], writes=[t_gm])
                S.op('dve', lambda e: e.tensor_scalar(out=nmqB, in0=gm, scalar1=m8B[:, 2:3], scalar2=NEGM, op0=ALU.is_lt, op1=ALU.mult), reads=[t_gm, t_m8], writes=[t_nmq])
                S.op('pe', lambda e, qb=qb: e.transpose(out=pbb[0:GW, qb * 128:(qb + 1) * 128], in_=nmqB, identity=identb), reads=[t_nmq, C.t_const], writes=[t_pbb])
            S.op('dve', lambda e, nl=nl: e.tensor_copy(out=nmTB[nl][0:GW], in_=pbb[0:GW, 0:512]), reads=[t_pbb], writes=[t_nmT[nl]])
            tiles = []
            for j in range(4 * s + 4):
                off = min(512 * s - 128 * j, 3072) + 384
                n = j // 2
                tiles.append(dict(kT=kT[0:64, j * 128:(j + 1) * 128], t_k=t_kT, v=vv[:, j, :], t_v=t_vv,
                                  extra=(ohBs[0:NKB, n * 128:(n + 1) * 128], nmTB[nl][0:NKB], t_nmT[nl]),
                                  band=bandt[:, off:off + 512], t_band=t_bandt))
            attn_tiles(S, C, qT[0:64, cs], t_qT, tiles)
            ol = oi % 2; oi += 1
            attn_finish(S, C, ooB[ol][0:64], t_oo[ol])
            out_evs.append(S.dma('pool', oT[256 + h * 64:256 + (h + 1) * 64, cs], ooB[ol][0:64], reads=[t_oo[ol]]))
    for h in range(4):
        S.dma('sp', qT[0:64], FMd[1536 + h * 64:1536 + (h + 1) * 64, :], writes=[t_qT])
        S.dma('sp', kT[0:64], FMd[1792 + h * 64:1792 + (h + 1) * 64, :], writes=[t_kT])
        load_v(vv, t_vv, 640 + h * 64)
        load_band(bandt, t_bandt, zd[h], WD_, mult=(cds, t_cds))
        for s in range(NS):
            cs = slice(s * 512, (s + 1) * 512)
            tiles = []
            for j in range(max(0, 4 * s - 16), 4 * s + 4):
                off = (512 * s - 128 * j) + 384
                tiles.append(dict(kT=kT[0:64, j * 128:(j + 1) * 128], t_k=t_kT, v=vv[:, j, :], t_v=t_vv,
                                  band=bandt[:, off:off + 512], t_band=t_bandt))
            attn_tiles(S, C, qT[0:64, cs], t_qT, tiles)
            ol = oi % 2; oi += 1
            attn_finish(S, C, ooB[ol][0:64], t_oo[ol])
            out_evs.append(S.dma('pool', oT[768 + h * 64:768 + (h + 1) * 64, cs], ooB[ol][0:64], reads=[t_oo[ol]]))
    m3 = _mark(S)
    NB4 = NB * 4
    lfT = S.sb([NB, 4], F32); t_lf = Trk()
    spb = S.sb([NB, 4], F32); t_sp = Trk()
    cT = S.sb([NB, 4], F32); t_cT = Trk()
    pfx = [S.sb([NB, 4], F32) for _ in range(2)]; t_pfx = [Trk(), Trk()]
    tot = S.sb([NB, 4], F32); t_tot = Trk()
    fb = S.sb([4], F32); nfb = S.sb([4], F32); t_fb = Trk()
    c0s = S.sb([NS, 4], F32); c0b = S.sb([NS, 4], F32); t_c0 = Trk()
    crelT = S.sb([NB, 4], F32); t_crel = Trk()
    crow = S.sb([T], F32); t_crow = Trk()
    chi = S.sb([T], BF16); clo = S.sb([T], BF16); t_chl = Trk()
    biasall = S.sb([NS, NB, 4], F32); t_ball = Trk()
    trif = S.sb([128], F32); r0f = S.sb([128], F32)
    S.dma('sp', trif, tri, writes=[C.t_const]); S.dma('sp', r0f, r0m, writes=[C.t_const])
    S.dma('sp', fb, foxb, writes=[t_fb])
    S.op('dve', lambda e: e.tensor_scalar(out=nfb, in0=fb, scalar1=-1.0, scalar2=None, op0=ALU.mult), reads=[t_fb], writes=[t_fb])
    S.dma('sp', lfT, FCd.rearrange("(j p) h -> p j h", p=128), writes=[t_lf])
    for h in range(4):
        S.op('act', lambda e, h=h: e.activation(out=spb[:, :, h], in_=lfT[:, :, h], func=AF.Exp, bias=nfb[:, h:h + 1], scale=-1.0), reads=[t_lf, t_fb], writes=[t_sp])
    S.op('act', lambda e: e.activation(out=spb, in_=spb, func=AF.Ln, bias=onecol, scale=1.0), reads=[t_sp, C.t_const], writes=[t_sp])
    spf = spb.rearrange("p j h -> p (j h)")
    S.op('pe', mm(banks[4][:, 0:NB4], trif, spf, True, True), reads=[t_sp, C.t_const], writes=[t_bank[4]])
    S.op('pe', mm(banks[5][:, 0:NB4], onesf, spf, True, True), reads=[t_sp, C.t_const], writes=[t_bank[5]])
    S.op('dve', lambda e: e.tensor_copy(out=tot.rearrange("p j h -> p (j h)"), in_=banks[5][:, 0:NB4]), reads=[t_bank[5]], writes=[t_tot])
    S.op('dve', lambda e: e.tensor_copy(out=pfx[0], in_=tot), reads=[t_tot], writes=[t_pfx[0]])
    cur_ = 0
    sh = 1
    while sh < NB:
        a, b_ = pfx[cur_], pfx[1 - cur_]
        S.op('dve', lambda e, a=a, b_=b_, sh=sh: e.tensor_copy(out=b_[:, 0:sh, :], in_=a[:, 0:sh, :]), reads=[t_pfx[cur_]], writes=[t_pfx[1 - cur_]])
        S.op('dve', lambda e, a=a, b_=b_, sh=sh: e.tensor_tensor(out=b_[:, sh:NB, :], in0=a[:, sh:NB, :], in1=a[:, 0:NB - sh, :], op=ALU.add), reads=[t_pfx[cur_]], writes=[t_pfx[1 - cur_]])
        cur_ = 1 - cur_
        sh *= 2
    incl = pfx[cur_]
    S.op('dve', lambda e: e.tensor_tensor(out=cT.rearrange("p j h -> p (j h)"), in0=banks[4][:, 0:NB4], in1=incl.rearrange("p j h -> p (j h)"), op=ALU.add), reads=[t_bank[4], t_pfx[cur_]], writes=[t_cT])
    S.op('dve', lambda e: e.tensor_tensor(out=cT, in0=tot, in1=cT, op=ALU.subtract), reads=[t_cT, t_tot], writes=[t_cT])
    for s in range(NS):
        S.op('dve', lambda e, s=s: e.tensor_copy(out=c0s[:, s, :], in_=cT[:, 4 * s, :]), reads=[t_cT], writes=[t_c0])
    S.op('pe', mm(banks[4][:, 0:NS * 4], r0f, c0s.rearrange("p s h -> p (s h)"), True, True), reads=[t_c0, C.t_const], writes=[t_bank[4]])
    S.op('dve', lambda e: e.tensor_copy(out=c0b.rearrange("p s h -> p (s h)"), in_=banks[4][:, 0:NS * 4]), reads=[t_bank[4]], writes=[t_c0])
    for j in range(NB):
        S.op('dve', lambda e, j=j: e.tensor_tensor(out=crelT[:, j, :], in0=cT[:, j, :], in1=c0b[:, j // 4, :], op=ALU.subtract), reads=[t_cT, t_c0], writes=[t_crel])
    for s in range(NS):
        for jj in range(4):
            j = 4 * s + jj
            S.op('pe', lambda e, j=j, jj=jj: e.transpose(out=banks[5][0:4, jj * 128:(jj + 1) * 128], in_=crelT[:, j, :], identity=identfs), reads=[t_crel, C.t_const], writes=[t_bank[5]])
        S.op('dve', lambda e, s=s: e.tensor_copy(out=crow[0:4, s * 512:(s + 1) * 512], in_=banks[5][0:4, 0:512]), reads=[t_bank[5]], writes=[t_crow])
    S.op('dve', lambda e: e.tensor_copy(out=chi[0:4], in_=crow[0:4]), reads=[t_crow], writes=[t_chl])
    S.op('dve', lambda e: e.tensor_tensor(out=crow[0:4], in0=crow[0:4], in1=chi[0:4], op=ALU.subtract), reads=[t_crow, t_chl], writes=[t_crow])
    S.op('dve', lambda e: e.tensor_copy(out=clo[0:4], in_=crow[0:4]), reads=[t_crow], writes=[t_chl])
    for s in range(NS):
        for h in range(4):
            nj = 4 * s + 4
            S.op('dve', lambda e, s=s, h=h, nj=nj: e.tensor_scalar(out=biasall[:, s, 0:nj, h], in0=cT[:, 0:nj, h], scalar1=-1.0, scalar2=c0b[:, s, h:h + 1], op0=ALU.mult, op1=ALU.add),
                 reads=[t_cT, t_c0], writes=[t_ball])
    for h in range(4):
        S.dma('sp', qT[0:64], FMd[1024 + h * 64:1024 + (h + 1) * 64, :], writes=[t_qT])
        S.dma('sp', qT[64:65], chi[h:h + 1], reads=[t_chl], writes=[t_qT])
        S.dma('sp', qT[65:66], clo[h:h + 1], reads=[t_chl], writes=[t_qT])
        S.dma('sp', kT[0:64], FMd[1280 + h * 64:1280 + (h + 1) * 64, :], writes=[t_kT])
        S.op('pool', lambda e: e.memset(kT[64:66], 1.0), writes=[t_kT])
        load_v(vv, t_vv, 384 + h * 64)
        for s in range(NS):
            cs = slice(s * 512, (s + 1) * 512)
            tiles = []
            for j in range(4 * s + 4):
                d = dict(kT=kT[0:66, j * 128:(j + 1) * 128], t_k=t_kT, v=vv[:, j, :], t_v=t_vv,
                         bias=biasall[:, s, j, h:h + 1], t_bias=t_ball)
                if j >= 4 * s:
                    off = (512 * s - 128 * j) + 384
                    d['band'] = ccs[:, off:off + 512]
                    d['t_band'] = t_ccs
                tiles.append(d)
            attn_tiles(S, C, qT[0:66, cs], t_qT, tiles)
            ol = oi % 2; oi += 1
            attn_finish(S, C, ooB[ol][0:64], t_oo[ol])
            out_evs.append(S.dma('pool', oT[512 + h * 64:512 + (h + 1) * 64, cs], ooB[ol][0:64], reads=[t_oo[ol]]))
    S.emit()
    return nc


def build_B(NT):
    NS = NT // 512
    nc = bass.Bass("TRN2", target_bir_lowering=False)

    def din(n, sh, dt=F32):
        return nc.dram_tensor(n, list(sh), dt, kind="ExternalInput").ap()
    oTin = din("oTin", [2048, NT], BF16); xTin = din("xTin", [2048, NT])
    wout = din("wout", [2048, 2048]); wup = din("wup", [2048, 8192]); wdn = din("wdn", [8192, 2048])
    gpost = din("gpost", [128, 16]); gmp = din("gmp", [128, 16]); gmq = din("gmq", [128, 16])
    xo = nc.dram_tensor("xo", [2048, NT], F32, kind="ExternalOutput").ap()
    S = Sched(nc)
    S.init_arena(200 * 1024)
    banks = [S.psum("pb%d" % i) for i in range(8)]
    t_bank = [Trk() for _ in range(8)]
    t_c = Trk()
    ones_bf = S.sb([128], BF16); epscol = S.sb([1], F32)
    S.op('pool', lambda e: e.memset(ones_bf, 1.0), writes=[t_c])
    S.op('pool', lambda e: e.memset(epscol, 1e-6), writes=[t_c])
    g1 = S.sb([16], F32); g2 = S.sb([16], F32); g3 = S.sb([16], F32)
    S.dma('sp', g1, gpost, writes=[t_c]); S.dma('sp', g2, gmp, writes=[t_c]); S.dma('sp', g3, gmq, writes=[t_c])
    bufA = S.sb([16, 512], F32); t_A = Trk()
    bufX = S.sb([16, 512], F32); t_X = Trk()
    bufH = S.sb([16, 512], BF16); t_H = Trk()
    aT = S.sb([64, 512], BF16); t_aT = Trk()
    sqt = [S.sb([512], BF16) for _ in range(4)]; t_sqt = [Trk() for _ in range(4)]
    rbs = [S.sb([512], F32) for _ in range(3)]; t_rbs = [Trk() for _ in range(3)]
    wst = [S.sb([2048], F32) for _ in range(2)]; t_wst = [Trk(), Trk()]
    wbf = [S.sb([2048], BF16) for _ in range(2)]; t_wbf = [Trk(), Trk()]
    tmpf = [S.sb([512], F32) for _ in range(2)]; t_tmpf = [Trk(), Trk()]
    st = Ctx(); st.wi = 0; st.sqi = 0; st.ti = 0
    out_evs = []

    def load_w(src_ap, shape3):
        sl = st.wi % 2; st.wi += 1
        a, b = shape3
        v32 = wst[sl].rearrange("p (a b) -> p a b", b=b)
        v16 = wbf[sl].rearrange("p (a b) -> p a b", b=b)
        S.dma('sp' if sl == 0 else 'act', v32, src_ap, writes=[t_wst[sl]])
        S.op('pool', lambda e: e.tensor_copy(out=wbf[sl], in_=wst[sl]), reads=[t_wst[sl]], writes=[t_wbf[sl]])
        return v16, t_wbf[sl]

    def ss_accum(bank, t_bk, src_ap, t_src, first, last, from_psum_t=None):
        q = st.sqi % 4; st.sqi += 1
        S.op('act', lambda e: e.activation(out=sqt[q], in_=src_ap, func=AF.Square), reads=[t_src], writes=[t_sqt[q]])
        S.op('pe', mm(bank, ones_bf, sqt[q], first, last), reads=[t_sqt[q], t_c], writes=[t_bk])

    for s in range(NS):
        cs = slice(s * 512, (s + 1) * 512)
        S.dma('sp', bufH, oTin[:, cs].rearrange("(c p) t -> p c t", p=128), writes=[t_H])
        S.dma('act', bufX, xTin[:, cs].rearrange("(c p) t -> p c t", p=128), writes=[t_X])
        for dc in range(16):
            wv, t_w = load_w(wout[:, dc * 128:(dc + 1) * 128].rearrange("(c p) m -> p c m", p=128), (16, 128))
            bk = dc % 4
            for kc in range(16):
                S.op('pe', mm(banks[bk], wv[:, kc, :], bufH[:, kc, :], kc == 0, kc == 15), reads=[t_w, t_H], writes=[t_bank[bk]])
            S.op('dve', lambda e, dc=dc, bk=bk: e.tensor_copy(out=bufA[:, dc, :], in_=banks[bk]), reads=[t_bank[bk]], writes=[t_A])
            ss_accum(banks[4], t_bank[4], bufA[:, dc, :], t_A, dc == 0, dc == 15)
        rstd_from_ps(S, banks[4], rbs[0], t_bank[4], t_rbs[0], epscol, t_c, 1.0 / 2048)
        for dc in range(16):
            S.op('dve', lambda e, dc=dc: e.scalar_tensor_tensor(out=bufA[:, dc, :], in0=bufA[:, dc, :], scalar=g1[:, dc:dc + 1], in1=rbs[0], op0=ALU.mult, op1=ALU.mult),
                 reads=[t_A, t_rbs[0], t_c], writes=[t_A])
            S.op('pool', lambda e, dc=dc: e.tensor_tensor(out=bufX[:, dc, :], in0=bufX[:, dc, :], in1=bufA[:, dc, :], op=ALU.add), reads=[t_A, t_X], writes=[t_X])
            S.op('dve', lambda e, dc=dc: e.tensor_scalar(out=bufH[:, dc, :], in0=bufX[:, dc, :], scalar1=g2[:, dc:dc + 1], scalar2=None, op0=ALU.mult), reads=[t_X, t_c], writes=[t_H])
            ss_accum(banks[5], t_bank[5], bufX[:, dc, :], t_X, dc == 0, dc == 15)
        rstd_from_ps(S, banks[5], rbs[1], t_bank[5], t_rbs[1], epscol, t_c, 1.0 / 2048)
        for f in range(64):
            wv, t_w = load_w(wup[:, f * 128:(f + 1) * 128].rearrange("(c p) m -> p c m", p=128), (16, 128))
            bk = f % 4
            for kc in range(16):
                S.op('pe', mm(banks[bk], wv[:, kc, :], bufH[:, kc, :], kc == 0, kc == 15), reads=[t_w, t_H], writes=[t_bank[bk]])
            tq = st.ti % 2; st.ti += 1
            S.op('dve', lambda e, bk=bk, tq=tq: e.scalar_tensor_tensor(out=tmpf[tq], in0=banks[bk], scalar=0.0, in1=rbs[1], op0=ALU.max, op1=ALU.mult),
                 reads=[t_bank[bk], t_rbs[1]], writes=[t_tmpf[tq]])
            S.op('act', lambda e, f=f, tq=tq: e.activation(out=aT[:, f, :], in_=tmpf[tq], func=AF.Square), reads=[t_tmpf[tq]], writes=[t_aT])
        for dg in range(4):
            for fg in range(16):
                wv, t_w = load_w(wdn[fg * 512:(fg + 1) * 512, dg * 512:(dg + 1) * 512].rearrange("(c p) m -> p c m", p=128), (4, 512))
                for c4 in range(4):
                    f = fg * 4 + c4
                    for d4 in range(4):
                        S.op('pe', mm(banks[d4], wv[:, c4, d4 * 128:(d4 + 1) * 128], aT[:, f, :], f == 0, f == 63), reads=[t_w, t_aT], writes=[t_bank[d4]])
            for d4 in range(4):
                dc = dg * 4 + d4
                S.op('dve', lambda e, dc=dc, d4=d4: e.tensor_copy(out=bufA[:, dc, :], in_=banks[d4]), reads=[t_bank[d4]], writes=[t_A])
                ss_accum(banks[6], t_bank[6], bufA[:, dc, :], t_A, dc == 0, dc == 15)
        rstd_from_ps(S, banks[6], rbs[2], t_bank[6], t_rbs[2], epscol, t_c, 1.0 / 2048)
        for dc in range(16):
            S.op('dve', lambda e, dc=dc: e.scalar_tensor_tensor(out=bufA[:, dc, :], in0=bufA[:, dc, :], scalar=g3[:, dc:dc + 1], in1=rbs[2], op0=ALU.mult, op1=ALU.mult),
                 reads=[t_A, t_rbs[2], t_c], writes=[t_A])
            S.op('pool', lambda e, dc=dc: e.tensor_tensor(out=bufA[:, dc, :], in0=bufA[:, dc, :], in1=bufX[:, dc, :], op=ALU.add), reads=[t_A, t_X], writes=[t_A])
        out_evs.append(S.dma('pool', xo[:, cs].rearrange("(c p) t -> p c t", p=128), bufA, reads=[t_A]))
    S.emit()
    return nc


def _t5_bucket_np(d):
    d = np.maximum(d, 0)
    rel = np.log(np.maximum(d, 1).astype(np.float32) / np.float32(16)) / np.float32(math.log(4096 / 16))
    large = np.minimum(16 + (rel * np.float32(16)).astype(np.int32), 31)
    return np.where(d < 16, d, large).astype(np.int64)


def _consts(T):
    NB, NSW = T // 128, T // 64
    NCB = max(1, T // 2048)
    NCMP = T // 16 - 1
    k = np.arange(128)[:, None]
    c = {}
    c['ident'] = np.eye(128, dtype=np.float32).astype(BFNP)
    c['identf'] = np.eye(128, dtype=np.float32)
    jk = np.arange(NB * 128)[None, :]
    c['ohA'] = (k == 2 * (jk // 128) + (jk % 128) // 64).astype(np.float32).astype(BFNP)
    n32 = np.arange(32)[:, None]
    c['ohB'] = (n32 == (np.arange(32 * 128)[None, :] // 128)).astype(np.float32).astype(BFNP)
    ov = np.zeros((128, NCB, NSW + 1), np.float32)
    for jc in range(NCB):
        cc = jc * 128 + np.arange(128)
        n = np.arange(NSW)[None, :]
        m = ((16 * cc[:, None]) < 64 * n + 64) & ((16 * cc[:, None] + 31) >= 64 * n) & (cc[:, None] < NCMP)
        ov[:, jc, :NSW] = m
        ov[:, jc, NSW] = 1.0
    c['ovl'] = ov.astype(BFNP)
    m = np.arange(2 * NSW)[None, :]
    npr = m - (NSW - 2)
    curp = (k >= 64).astype(np.int64)
    valid = npr <= curp
    forced = (npr == curp) | (npr == curp - 1)
    c['vmb'] = (valid & ~forced).astype(np.float32)
    c['amb'] = np.where(forced, np.float32(BIGV), np.where(valid, np.float32(0), np.float32(-BIGV))).astype(np.float32)
    c['tri'] = (k <= np.arange(128)[None, :]).astype(np.float32)
    r0 = np.zeros((128, 128), np.float32); r0[0, :] = 1.0
    c['r0m'] = r0
    dist_t = np.arange(WT_)[None, :] - 384 - k
    dist_w = np.arange(WW_)[None, :] - 384 - k
    dist_c = np.arange(WC_)[None, :] - 31 - 16 * k
    dist_d = np.arange(WD_)[None, :] - 384 - k
    mult = (((dist_d >= 0) & (dist_d <= 128)).astype(np.int64)
            + ((dist_d >= 0) & (dist_d <= 512) & (dist_d % 4 == 0))
            + ((dist_d >= 0) & (dist_d <= 2048) & (dist_d % 16 == 0)))
    c['cd'] = mult.astype(np.float32).astype(BFNP)
    c['ccm'] = ((np.arange(896)[None, :] - 384 - k) >= 0).astype(np.float32).astype(BFNP)
    c['_maps'] = dict(t=(dist_t, dist_t >= 0), w=(dist_w, (dist_w >= 0) & (dist_w <= 511)),
                      c=(dist_c, dist_c >= 0), d=(dist_d, mult > 0))
    return c


def _band(tabh, dist, valid):
    idx = _t5_bucket_np(np.clip(dist, 0, None))
    out = tabh[idx].astype(np.float32)
    out[~valid] = np.float32(NEGM)
    return out


_PROJ_OFF = dict(qa=0, kca=512, vca=640, ksa=768, vsa=896, kwa=1024, vwa=1152, ga=1280, qb=1304, kb=1816, vb=2328,
                 qc=2840, kc=3352, vc=3864, fc=4376, qd=4384, kd=4896, vd=5408)


def _cols(g):
    P = _PROJ_OFF
    r256 = np.arange(256)
    r64 = np.arange(64)
    fm = [P['qa'] + g * 256 + r256, P['kca'] + g * 64 + r64, P['vca'] + g * 64 + r64, P['ksa'] + g * 64 + r64, P['kwa'] + g * 64 + r64,
          P['qb'] + g * 256 + r256, P['kb'] + g * 256 + r256, P['qc'] + g * 256 + r256, P['kc'] + g * 256 + r256,
          P['qd'] + g * 256 + r256, P['kd'] + g * 256 + r256,
          np.array([P['ga'] + b3 * 8 + g * 4 + r for b3 in range(3) for r in range(4)])]
    tm = [P['vsa'] + g * 64 + r64, P['vwa'] + g * 64 + r64, P['vb'] + g * 256 + r256, P['vc'] + g * 256 + r256, P['vd'] + g * 256 + r256,
          P['fc'] + g * 4 + np.arange(4)]
    return np.concatenate(fm + tm)


def _pm(v):
    return np.ascontiguousarray(np.asarray(v, np.float32).reshape(16, 128).T)


_NC_CACHE = {}


def _run_model(inp, B, T, depth):
    f32 = lambda a: np.asarray(a, dtype=np.float32)
    x = f32(inp['x'])
    rel_bias = f32(inp['rel_bias'])
    cst = _consts(T)
    maps = cst.pop('_maps')
    ncores = 2 * B
    if ('A', T) not in _NC_CACHE:
        _NC_CACHE[('A', T)] = build_A(T)
        _NC_CACHE[('B', T)] = build_B(T // 2)
    ncA, ncB = _NC_CACHE[('A', T)], _NC_CACHE[('B', T)]
    bands = []
    for g in range(2):
        d = {}
        d['zta'] = np.stack([_band(rel_bias[:, 4 * g + r], *maps['t']) for r in range(4)])
        d['zw'] = np.stack([_band(rel_bias[:, 4 * g + r], *maps['w']) for r in range(4)])
        d['zc'] = np.stack([_band(rel_bias[:, 4 * g + r], *maps['c']) for r in range(4)])
        d['ztb'] = np.stack([_band(rel_bias[:, 8 + 4 * g + r], *maps['t']) for r in range(4)])
        d['zd'] = np.stack([_band(rel_bias[:, 16 + 4 * g + r], *maps['d']) for r in range(4)])
        bands.append(d)
    xT = [np.ascontiguousarray(x[b].T) for b in range(B)]
    H = T // 2
    for l in range(depth):
        w_in = f32(inp['w_in'][l])
        in_maps = []
        for c in range(ncores):
            b, g = c // 2, c % 2
            m = dict(cst)
            m.update(bands[g])
            m['xT'] = xT[b]
            m['w'] = np.ascontiguousarray(w_in[:, _cols(g)])
            m['gpre'] = _pm(inp['g_mix_pre'][l])
            m['foxb'] = np.ascontiguousarray(np.broadcast_to(f32(inp['fox_bias'][l])[4 * g:4 * g + 4][None, :], (128, 4)))
            m['w1k'] = f32(inp['phik_w1'][l]); m['w2k'] = f32(inp['phik_w2'][l])
            m['w1v'] = f32(inp['phiv_w1'][l]); m['w2v'] = f32(inp['phiv_w2'][l])
            m['peT'] = np.ascontiguousarray(f32(inp['cmp_pe'][l]).T)
            in_maps.append(m)
        resA = run_bass_kernel_spmd(ncA, in_maps, core_ids=list(range(ncores)))
        oTs = [np.asarray(r['oT']) for r in resA.results]
        in_maps = []
        for c in range(ncores):
            b, hf = c // 2, c % 2
            o_full = np.empty((2048, H), dtype=oTs[0].dtype)
            for mixer in range(4):
                for g in range(2):
                    o_full[mixer * 512 + g * 256:mixer * 512 + (g + 1) * 256, :] = oTs[2 * b + g][mixer * 256:(mixer + 1) * 256, hf * H:(hf + 1) * H]
            in_maps.append(dict(oTin=o_full, xTin=np.ascontiguousarray(xT[b][:, hf * H:(hf + 1) * H]),
                                wout=f32(inp['w_out'][l]), wup=f32(inp['w_up'][l]), wdn=f32(inp['w_down'][l]),
                                gpost=_pm(inp['g_mix_post'][l]), gmp=_pm(inp['g_mlp_pre'][l]), gmq=_pm(inp['g_mlp_post'][l])))
        resB = run_bass_kernel_spmd(ncB, in_maps, core_ids=list(range(ncores)))
        for c in range(ncores):
            b, hf = c // 2, c % 2
            xT[b][:, hf * H:(hf + 1) * H] = np.asarray(resB.results[c]['xo'])
    return np.stack([np.ascontiguousarray(xT[b].T) for b in range(B)]).astype(np.float32)


def kernel(x, w_in, w_out, g_mix_pre, g_mix_post, g_mlp_pre, g_mlp_post, w_up, w_down,
           cmp_pe, phik_w1, phik_w2, phiv_w1, phiv_w2, fox_bias, rel_bias):
    inp = dict(x=x, w_in=w_in, w_out=w_out, g_mix_pre=g_mix_pre, g_mix_post=g_mix_post, g_mlp_pre=g_mlp_pre,
               g_mlp_post=g_mlp_post, w_up=w_up, w_down=w_down, cmp_pe=cmp_pe, phik_w1=phik_w1, phik_w2=phik_w2,
               phiv_w1=phiv_w1, phiv_w2=phiv_w2, fox_bias=fox_bias, rel_bias=rel_bias)
    xs = np.asarray(x)
    return _run_model(inp, xs.shape[0], xs.shape[1], np.asarray(w_in).shape[0])
```

```python
import math
import numpy as np
import ml_dtypes
from contextlib import ExitStack
import concourse.bass as bass
import concourse.mybir as mybir
from concourse.bass_utils import run_bass_kernel_spmd

F32 = mybir.dt.float32
BF16 = mybir.dt.bfloat16
U8 = mybir.dt.uint8
AF = mybir.ActivationFunctionType
ALU = mybir.AluOpType
AX = mybir.AxisListType


class Trk:
    __slots__ = ('w', 'r', 'ro')

    def __init__(self, ro=False):
        self.w = None
        self.r = {}
        self.ro = ro


class Sched:
    ENG = ['pe', 'act', 'dve', 'pool', 'sp']

    def __init__(self, nc, ndma=8):
        self.nc = nc
        self.ops = {e: [] for e in self.ENG}
        self.cnt = {e: 0 for e in self.ENG}
        self.seen = {e: {} for e in self.ENG}
        self.dma_i = {e: 0 for e in self.ENG}
        self.ndma = ndma
        self.semvals = {}
        self.es = ExitStack()
        self.arena = None
        self.arena_off = 0
        self.arena_size = 0

    def init_arena(self, nbytes):
        self.arena = self.es.enter_context(self.nc.sbuf_tensor("arena", [128, nbytes], U8))
        self.arena_size = nbytes
        self.arena_off = 0

    def sb(self, shape, dtype, parts=128):
        n = int(np.prod(shape))
        esz = 4 if dtype == F32 else (2 if dtype == BF16 else 1)
        nb = (n * esz + 31) // 32 * 32
        assert self.arena_off + nb <= self.arena_size, ("arena overflow", self.arena_off, nb)
        v = self.arena[:, self.arena_off:self.arena_off + n * esz]
        self.arena_off += nb
        if dtype != U8:
            v = v.bitcast(dtype)
        if len(shape) == 2:
            v = v.rearrange("p (a b) -> p a b", b=shape[1])
        elif len(shape) == 3:
            v = v.rearrange("p (a b c) -> p a b c", b=shape[1], c=shape[2])
        return v

    def psum(self, name, shape=(128, 512), dtype=F32):
        return self.es.enter_context(self.nc.psum_tensor(name, list(shape), dtype)).ap()

    def _waits(self, eng, reads, writes):
        need = {}

        def add(s, v):
            if s == 'pe' and eng == 'pe':
                return
            if self.seen[eng].get(s, 0) >= v:
                return
            if need.get(s, 0) < v:
                need[s] = v
        for t in reads:
            if t.w is not None:
                add(*t.w)
        for t in writes:
            if t.w is not None:
                add(*t.w)
            for s, v in t.r.items():
                add(s, v)
        for s, v in need.items():
            self.seen[eng][s] = v
            self.ops[eng].append(('w', s, v))

    def _commit(self, ev, reads, writes):
        s, v = ev
        for t in reads:
            if not t.ro:
                if t.r.get(s, 0) < v:
                    t.r[s] = v
        for t in writes:
            t.w = ev
            t.r = {}
        if self.semvals.get(s, 0) < v:
            self.semvals[s] = v

    def op(self, eng, fn, reads=(), writes=()):
        self._waits(eng, reads, writes)
        self.cnt[eng] += 1
        ev = (eng, self.cnt[eng])
        self.ops[eng].append(('i', fn, eng, 1))
        self._commit(ev, reads, writes)
        return ev

    def dma(self, q, out, in_, reads=(), writes=(), **kw):
        i = self.dma_i[q]
        self.dma_i[q] += 1
        sname = 'd_%s_%d' % (q, i % self.ndma)
        prev = 16 * (i // self.ndma)
        if prev > 0 and self.seen[q].get(sname, 0) < prev:
            self.seen[q][sname] = prev
            self.ops[q].append(('w', sname, prev))
        self._waits(q, reads, writes)
        ev = (sname, prev + 16)
        self.ops[q].append(('i', (lambda e, out=out, in_=in_, kw=kw: e.dma_start(out=out, in_=in_, **kw)), sname, 16))
        self._commit(ev, reads, writes)
        return ev

    def barrier(self):
        for e in self.ENG:
            for s, v in self.semvals.items():
                if s == e and e == 'pe':
                    continue
                if self.seen[e].get(s, 0) < v:
                    self.seen[e][s] = v
                    self.ops[e].append(('w', s, v))

    def emit(self):
        nc = self.nc
        self.barrier()
        sems = {}
        for s in self.semvals:
            sems[s] = self.es.enter_context(nc.semaphore(s))
        engmap = {'pe': 'tensor', 'act': 'scalar', 'dve': 'vector', 'pool': 'gpsimd', 'sp': 'sync'}
        with nc.Block() as block:
            for e in self.ENG:
                ops = self.ops[e]

                def body(eng, ops=ops):
                    for o in ops:
                        if o[0] == 'w':
                            eng.wait_ge(sems[o[1]], o[2])
                        else:
                            ins = o[1](eng)
                            ins.then_inc(sems[o[2]], o[3])
                getattr(block, engmap[e])(body)
        self.es.close()

BFNP = ml_dtypes.bfloat16
NEGM = -30000.0
BIGV = 1e30
WT_, WW_, WC_, WD_ = 3968, 1408, 5632, 2944


def mm(out, l, r, st, sp):
    return lambda e: e.matmul(out, lhsT=l, rhs=r, start=st, stop=sp)


class Ctx:
    pass


def _mark(S):
    return S.arena_off


def _reset(S, off):
    S.arena_off = off


def rstd_from_ps(S, ps_ap, out_ap, t_ps, t_out, epscol, t_c, scale):
    S.op('act', lambda e: e.activation(out=out_ap, in_=ps_ap, func=AF.Ln, bias=epscol, scale=scale),
         reads=[t_ps, t_c], writes=[t_out])
    S.op('act', lambda e: e.activation(out=out_ap, in_=out_ap, func=AF.Exp, scale=-0.5),
         reads=[t_out], writes=[t_out])


def attn_tiles(S, C, qrhs, t_q, tiles, KQ=64):
    n = len(tiles)
    for i, tl in enumerate(tiles):
        b = C.tctr % 2
        C.tctr += 1
        st, t_st = C.st[b], C.t_st[b]
        pe_, t_pe = C.pebuf[b], C.t_pebuf[b]
        pt_, t_pt = C.ptbuf[b], C.t_ptbuf[b]
        ex = tl.get('extra')
        S.op('pe', mm(st, tl['kT'], qrhs, True, ex is None), reads=[tl['t_k'], t_q], writes=[t_st])
        if ex is not None:
            S.op('pe', mm(st, ex[0], ex[1], False, True), reads=[ex[2], C.t_const], writes=[t_st])
        bias = tl.get('bias')
        if bias is not None:
            S.op('act', lambda e, pe_=pe_, st=st, bias=bias: e.activation(out=pe_, in_=st, func=AF.Exp, bias=bias, scale=1.0),
                 reads=[t_st, tl['t_bias']], writes=[t_pe])
        else:
            S.op('act', lambda e, pe_=pe_, st=st: e.activation(out=pe_, in_=st, func=AF.Exp), reads=[t_st], writes=[t_pe])
        band = tl.get('band')
        if band is not None:
            S.op('dve', lambda e, pt_=pt_, pe_=pe_, band=band: e.tensor_tensor(out=pt_, in0=pe_, in1=band, op=ALU.mult),
                 reads=[t_pe, tl['t_band']], writes=[t_pt])
            P, t_P = pt_, t_pt
        else:
            P, t_P = pe_, t_pe
        keep = tl.get('keep')
        if keep is not None:
            S.op('pool', lambda e, k=keep[0], P=P: e.tensor_copy(out=k, in_=P), reads=[t_P], writes=[keep[1]])
        S.op('pe', mm(C.num[0:64, :], tl['v'], P, i == 0, i == n - 1), reads=[tl['t_v'], t_P], writes=[C.t_num])
        S.op('pe', mm(C.den[0:64, :], C.ones_bf[:, 0:64], P, i == 0, i == n - 1), reads=[C.t_const, t_P], writes=[C.t_den])


def attn_finish(S, C, out_ap, t_out):
    S.op('dve', lambda e: e.tensor_scalar(out=C.rd[0:64], in0=C.den[0:64, :], scalar1=1e-30, scalar2=None, op0=ALU.max), reads=[C.t_den], writes=[C.t_rd])
    S.op('dve', lambda e: e.reciprocal(out=C.rd[0:64], in_=C.rd[0:64]), reads=[C.t_rd], writes=[C.t_rd])
    S.op('dve', lambda e, o=out_ap: e.tensor_tensor(out=o, in0=C.num[0:64, :], in1=C.rd[0:64], op=ALU.mult),
         reads=[C.t_num, C.t_rd], writes=[t_out])


def build_A(T):
    NS, NB, NKB = T // 512, T // 128, T // 256
    NSW = T // 64
    NCB = max(1, T // 2048)
    NCMP = T // 16 - 1
    nc = bass.Bass("TRN2", target_bir_lowering=False)

    def din(n, sh, dt=F32):
        return nc.dram_tensor(n, list(sh), dt, kind="ExternalInput").ap()

    def dsc(n, sh, dt=F32):
        return nc.dram_tensor(n, list(sh), dt, kind="Internal").ap()
    xT = din("xT", [2048, T]); w = din("w", [2048, 2960]); gpre = din("gpre", [128, 16]); foxb = din("foxb", [128, 4])
    zta = din("zta", [4, 128, WT_]); ztb = din("ztb", [4, 128, WT_]); zw = din("zw", [4, 128, WW_])
    zc = din("zc", [4, 128, WC_]); zd = din("zd", [4, 128, WD_])
    cd = din("cd", [128, WD_], BF16); ccm = din("ccm", [128, 896], BF16)
    ident = din("ident", [128, 128], BF16); identf = din("identf", [128, 128])
    ohA = din("ohA", [128, NB * 128], BF16); ohB = din("ohB", [32, 32 * 128], BF16)
    ovl = din("ovl", [128, NCB, NSW + 1], BF16)
    vmb = din("vmb", [128, 2 * NSW]); amb = din("amb", [128, 2 * NSW])
    tri = din("tri", [128, 128]); r0m = din("r0m", [128, 128])
    w1k = din("w1k", [2048, 256]); w2k = din("w2k", [256, 64]); w1v = din("w1v", [2048, 256]); w2v = din("w2v", [256, 64])
    peT = din("peT", [64, 32])
    oT = nc.dram_tensor("oT", [1024, T], BF16, kind="ExternalOutput").ap()
    FMd = dsc("FMd", [2048, T], BF16); GAd = dsc("GAd", [12, T]); GSd = dsc("GSd", [12, T])
    TMd = dsc("TMd", [T, 896], BF16); FCd = dsc("FCd", [T, 4])
    OCd = dsc("OCd", [256, T]); NMd = dsc("NMd", [NS, 128, 512], BF16)

    S = Sched(nc)
    S.init_arena(200 * 1024)
    banks = [S.psum("pb%d" % i) for i in range(7)]
    t_bank = [Trk() for _ in range(7)]
    pbb = S.psum("pbb", (128, 1024), BF16)
    t_pbb = Trk()
    out_evs = []

    C = Ctx()
    C.t_const = Trk()
    C.ones_bf = S.sb([128], BF16)
    onesf = S.sb([128], F32)
    epscol = S.sb([1], F32)
    onecol = S.sb([1], F32)
    S.op('pool', lambda e: e.memset(C.ones_bf, 1.0), writes=[C.t_const])
    S.op('pool', lambda e: e.memset(onesf, 1.0), writes=[C.t_const])
    S.op('pool', lambda e: e.memset(epscol, 1e-6), writes=[C.t_const])
    S.op('pool', lambda e: e.memset(onecol, 1.0), writes=[C.t_const])
    gp = S.sb([16], F32)
    S.dma('sp', gp, gpre, writes=[C.t_const])
    base0 = _mark(S)

    Wb = S.sb([16, 2960], BF16); t_W = Trk()
    m1 = _mark(S)
    wst = [S.sb([2960], F32) for _ in range(2)]; t_wst = [Trk(), Trk()]
    for kc in range(16):
        sl = kc % 2
        S.dma('sp', wst[sl], w[kc * 128:(kc + 1) * 128, :], writes=[t_wst[sl]])
        eng = 'dve' if kc % 2 == 0 else 'pool'
        S.op(eng, lambda e, kc=kc, sl=sl: e.tensor_copy(out=Wb[:, kc, :], in_=wst[sl]), reads=[t_wst[sl]], writes=[t_W])
    S.barrier()
    _reset(S, m1)
    xs = [S.sb([4, 512], F32) for _ in range(2)]; t_xs = [Trk(), Trk()]
    hb = [S.sb([16, 512], BF16) for _ in range(2)]; t_hb = [Trk(), Trk()]
    sq = [S.sb([16, 512], BF16) for _ in range(2)]; t_sq = [Trk(), Trk()]
    rb = [S.sb([512], F32) for _ in range(2)]; t_rb = [Trk(), Trk()]
    rt = [S.sb([4], F32) for _ in range(2)]; t_rt = [Trk(), Trk()]
    ob = [S.sb([512], BF16) for _ in range(4)]; t_ob = [Trk() for _ in range(4)]
    obf = S.sb([512], F32); t_obf = Trk()
    tmo = [S.sb([450], BF16) for _ in range(2)]; t_tmo = [Trk(), Trk()]
    fco = [S.sb([4], F32) for _ in range(2)]; t_fco = [Trk(), Trk()]
    QCH = (0, 1, 4, 5, 8, 9, 12, 13)
    xi = 0
    for s in range(NS):
        sl = s % 2
        cs = slice(s * 512, (s + 1) * 512)
        for kg in range(4):
            xl = xi % 2; xi += 1
            S.dma('sp', xs[xl], xT[kg * 512:(kg + 1) * 512, cs].rearrange("(c p) t -> p c t", p=128), writes=[t_xs[xl]])
            for c4 in range(4):
                kc = kg * 4 + c4
                S.op('dve', lambda e, kc=kc, c4=c4, xl=xl, sl=sl: e.tensor_scalar(out=hb[sl][:, kc, :], in0=xs[xl][:, c4, :], scalar1=gp[:, kc:kc + 1], scalar2=None, op0=ALU.mult),
                     reads=[t_xs[xl], C.t_const], writes=[t_hb[sl]])
            S.op('act', lambda e, kg=kg, xl=xl, sl=sl: e.activation(out=sq[sl][:, kg * 4:(kg + 1) * 4, :], in_=xs[xl], func=AF.Square),
                 reads=[t_xs[xl]], writes=[t_sq[sl]])
        for kc in range(16):
            S.op('pe', mm(banks[4], C.ones_bf, sq[sl][:, kc, :], kc == 0, kc == 15), reads=[t_sq[sl], C.t_const], writes=[t_bank[4]])
        rstd_from_ps(S, banks[4], rb[sl], t_bank[4], t_rb[sl], epscol, C.t_const, 1.0 / 2048)
        for tb in range(4):
            for kc in range(16):
                S.op('pe', mm(banks[5][:, tb:tb + 1], sq[sl][:, kc, tb * 128:(tb + 1) * 128], C.ones_bf[:, 0:1], kc == 0, kc == 15),
                     reads=[t_sq[sl], C.t_const], writes=[t_bank[5]])
        rstd_from_ps(S, banks[5][:, 0:4], rt[sl], t_bank[5], t_rt[sl], epscol, C.t_const, 1.0 / 2048)
        for cc in range(17):
            M = 128 if cc < 16 else 12
            bk = cc % 4
            for kc in range(16):
                S.op('pe', mm(banks[bk][0:M, :], Wb[:, kc, cc * 128:cc * 128 + M], hb[sl][:, kc, :], kc == 0, kc == 15),
                     reads=[t_W, t_hb[sl]], writes=[t_bank[bk]])
            if cc < 16:
                o_, t_o = ob[cc % 4], t_ob[cc % 4]
                if cc in QCH:
                    S.op('dve', lambda e, o_=o_, bk=bk, sl=sl: e.scalar_tensor_tensor(out=o_, in0=banks[bk], scalar=0.125, in1=rb[sl], op0=ALU.mult, op1=ALU.mult),
                         reads=[t_bank[bk], t_rb[sl]], writes=[t_o])
                else:
                    S.op('dve', lambda e, o_=o_, bk=bk, sl=sl: e.tensor_tensor(out=o_, in0=banks[bk], in1=rb[sl], op=ALU.mult),
                         reads=[t_bank[bk], t_rb[sl]], writes=[t_o])
                S.dma('pool', FMd[cc * 128:(cc + 1) * 128, cs], o_, reads=[t_o])
            else:
                S.op('dve', lambda e, bk=bk, sl=sl: e.tensor_tensor(out=obf[0:12], in0=banks[bk][0:12, :], in1=rb[sl][0:12], op=ALU.mult),
                     reads=[t_bank[bk], t_rb[sl]], writes=[t_obf])
                S.dma('pool', GAd[:, cs], obf[0:12], reads=[t_obf])
        for tb in range(4):
            for cg in range(2):
                bk = 5 + ((tb * 2 + cg) % 2)
                if bk == 5:
                    bk = 4
                for kc in range(16):
                    S.op('pe', mm(banks[bk][:, 0:450], hb[sl][:, kc, tb * 128:(tb + 1) * 128], Wb[:, kc, 2060 + cg * 450:2060 + (cg + 1) * 450], kc == 0, kc == 15),
                         reads=[t_W, t_hb[sl]], writes=[t_bank[bk]])
                tl = (tb * 2 + cg) % 2
                rows = slice(s * 512 + tb * 128, s * 512 + (tb + 1) * 128)
                if cg == 0:
                    S.op('act', lambda e, bk=bk, tl=tl, tb=tb, sl=sl: e.activation(out=tmo[tl], in_=banks[bk][:, 0:450], func=AF.Copy, scale=rt[sl][:, tb:tb + 1]),
                         reads=[t_bank[bk], t_rt[sl]], writes=[t_tmo[tl]])
                    S.dma('pool', TMd[rows, 0:450], tmo[tl], reads=[t_tmo[tl]])
                else:
                    S.op('act', lambda e, bk=bk, tl=tl, tb=tb, sl=sl: e.activation(out=tmo[tl][:, 0:446], in_=banks[bk][:, 0:446], func=AF.Copy, scale=rt[sl][:, tb:tb + 1]),
                         reads=[t_bank[bk], t_rt[sl]], writes=[t_tmo[tl]])
                    S.op('act', lambda e, bk=bk, tl=tl, tb=tb, sl=sl: e.activation(out=fco[tb % 2], in_=banks[bk][:, 446:450], func=AF.Copy, scale=rt[sl][:, tb:tb + 1]),
                         reads=[t_bank[bk], t_rt[sl]], writes=[t_fco[tb % 2]])
                    S.dma('pool', TMd[rows, 450:896], tmo[tl][:, 0:446], reads=[t_tmo[tl]])
                    S.dma('pool', FCd[rows, :], fco[tb % 2], reads=[t_fco[tb % 2]])
    S.barrier()
    _reset(S, base0)

    identb = S.sb([128], BF16); identfs = S.sb([128], F32)
    S.dma('sp', identb, ident, writes=[C.t_const])
    S.dma('sp', identfs, identf, writes=[C.t_const])
    C.st = [banks[0], banks[1]]; C.t_st = [t_bank[0], t_bank[1]]
    C.num, C.t_num = banks[2], t_bank[2]
    C.den, C.t_den = banks[3], t_bank[3]
    C.pebuf = [S.sb([512], BF16) for _ in range(2)]; C.t_pebuf = [Trk(), Trk()]
    C.ptbuf = [S.sb([512], BF16) for _ in range(2)]; C.t_ptbuf = [Trk(), Trk()]
    C.rd = S.sb([512], F32); C.t_rd = Trk()
    C.tctr = 0
    zst = S.sb([WC_], F32); t_zst = Trk()
    base1 = _mark(S)

    def load_band(dst, t_dst, src, W, mult=None):
        S.dma('sp', zst[:, 0:W], src, writes=[t_zst])
        S.op('act', lambda e: e.activation(out=dst[:, 0:W], in_=zst[:, 0:W], func=AF.Exp), reads=[t_zst], writes=[t_dst])
        if mult is not None:
            S.op('dve', lambda e: e.tensor_tensor(out=dst[:, 0:W], in0=dst[:, 0:W], in1=mult[0], op=ALU.mult), reads=[t_dst, mult[1]], writes=[t_dst])

    def load_v(dst, t_dst, col):
        step = 8
        for j0 in range(0, NB, step):
            j1 = min(NB, j0 + step)
            S.dma('sp', dst[:, j0:j1, :], TMd[j0 * 128:j1 * 128, col:col + 64].rearrange("(j p) d -> p j d", p=128), writes=[t_dst])

    w1b = S.sb([32, 256], BF16); t_w1 = Trk()
    w2kb = S.sb([2, 64], BF16); w2vb = S.sb([2, 64], BF16); t_w2 = Trk()
    pebf = S.sb([32], BF16); t_pe32 = Trk()
    kxT = S.sb([T], BF16); t_kx = Trk()
    hm = S.sb([2, 512 * NCB], BF16); t_hm = Trk()
    kcT = S.sb([128 * NCB], BF16); t_kcT = Trk()
    vcp = S.sb([NCB, 64], BF16); t_vcp = Trk()
    pbias = S.sb([2], F32); t_pbias = Trk()
    g1 = S.sb([512], F32); g2 = S.sb([512], F32); t_g1 = Trk(); t_g2 = Trk()
    m2 = _mark(S)
    w1st = S.sb([32, 256], F32); t_w1st = Trk()
    w2st = S.sb([2, 64], F32); t_w2st = Trk()
    pest = S.sb([32], F32)
    S.dma('sp', pest[0:64], peT, writes=[t_pe32])
    S.op('dve', lambda e: e.tensor_copy(out=pebf[0:64], in_=pest[0:64]), reads=[t_pe32], writes=[t_pe32])
    NCOL = NCMP
    for which in range(2):
        w1src = w1k if which == 0 else w1v
        w2src = w2k if which == 0 else w2v
        w2b = w2kb if which == 0 else w2vb
        for i0 in range(0, 32, 8):
            S.dma('sp', w1st[0:64, i0:i0 + 8, :], w1src[i0 * 64:(i0 + 8) * 64, :].rearrange("(i d) h -> d i h", d=64), writes=[t_w1st])
        S.op('dve', lambda e: e.tensor_copy(out=w1b[0:64], in_=w1st[0:64]), reads=[t_w1st], writes=[t_w1])
        S.dma('sp', w2st, w2src.rearrange("(c p) d -> p c d", p=128), writes=[t_w2st])
        S.op('dve', lambda e, w2b=w2b: e.tensor_copy(out=w2b, in_=w2st), reads=[t_w2st], writes=[t_w2])
        S.dma('sp', kxT[0:64], FMd[256 + which * 64:320 + which * 64, :], writes=[t_kx])
        S.op('pool', lambda e: e.memset(hm, 0.0), writes=[t_hm])
        for hc in range(2):
            for i in range(32):
                S.op('pe', mm(banks[4][:, 0:1], w1b[0:64, i, hc * 128:(hc + 1) * 128], pebf[0:64, i:i + 1], i == 0, i == 31),
                     reads=[t_w1, t_pe32], writes=[t_bank[4]])
            S.op('dve', lambda e, hc=hc: e.tensor_copy(out=pbias[:, hc:hc + 1], in_=banks[4][:, 0:1]), reads=[t_bank[4]], writes=[t_pbias])
            c0 = 0
            while c0 < NCOL:
                N = min(512, NCOL - c0)
                for i in range(32):
                    S.op('pe', mm(banks[5][:, 0:N], w1b[0:64, i, hc * 128:(hc + 1) * 128], kxT[0:64, i + 16 * c0:i + 16 * (c0 + N - 1) + 1:16], i == 0, i == 31),
                         reads=[t_w1, t_kx], writes=[t_bank[5]])
                S.op('act', lambda e, hc=hc, N=N: e.activation(out=g1[:, 0:N], in_=banks[5][:, 0:N], func=AF.Identity, bias=pbias[:, hc:hc + 1], scale=1.0),
                     reads=[t_bank[5], t_pbias], writes=[t_g1])
                S.op('dve', lambda e, N=N: e.tensor_tensor(out=g2[:, 0:N], in0=g1[:, 0:N], in1=g1[:, 0:N], op=ALU.mult), reads=[t_g1], writes=[t_g2])
                S.op('dve', lambda e, N=N: e.tensor_scalar(out=g2[:, 0:N], in0=g2[:, 0:N], scalar1=0.044715, scalar2=1.0, op0=ALU.mult, op1=ALU.add), reads=[t_g2], writes=[t_g2])
                S.op('dve', lambda e, N=N: e.tensor_tensor(out=g2[:, 0:N], in0=g2[:, 0:N], in1=g1[:, 0:N], op=ALU.mult), reads=[t_g1, t_g2], writes=[t_g2])
                S.op('act', lambda e, N=N: e.activation(out=g2[:, 0:N], in_=g2[:, 0:N], func=AF.Sigmoid, scale=2.0 * math.sqrt(2.0 / math.pi)), reads=[t_g2], writes=[t_g2])
                S.op('dve', lambda e, hc=hc, N=N, c0=c0: e.tensor_tensor(out=hm[:, hc, c0:c0 + N], in0=g2[:, 0:N], in1=g1[:, 0:N], op=ALU.mult), reads=[t_g1, t_g2], writes=[t_hm])
                c0 += N
        if which == 0:
            S.op('pool', lambda e: e.memset(kcT, 0.0), writes=[t_kcT])
            c0 = 0
            while c0 < NCOL:
                N = min(512, NCOL - c0)
                for hc in range(2):
                    S.op('pe', mm(banks[4][0:64, 0:N], w2kb[:, hc, :], hm[:, hc, c0:c0 + N], hc == 0, hc == 1), reads=[t_w2, t_hm], writes=[t_bank[4]])
                S.op('dve', lambda e, c0=c0, N=N: e.tensor_copy(out=kcT[0:64, c0:c0 + N], in_=banks[4][0:64, 0:N]), reads=[t_bank[4]], writes=[t_kcT])
                c0 += N
        else:
            for cb in range(NCB):
                for hc in range(2):
                    S.op('pe', mm(banks[4][:, 0:64], hm[:, hc, cb * 128:(cb + 1) * 128], w2vb[:, hc, :], hc == 0, hc == 1), reads=[t_w2, t_hm], writes=[t_bank[4]])
                S.op('dve', lambda e, cb=cb: e.tensor_copy(out=vcp[:, cb, :], in_=banks[4][:, 0:64]), reads=[t_bank[4]], writes=[t_vcp])
    S.barrier()
    _reset(S, m2)
    ohAs = S.sb([NB * 128], BF16)
    S.dma('sp', ohAs, ohA, writes=[C.t_const])
    ovls = S.sb([NCB, NSW + 1], BF16)
    S.dma('sp', ovls, ovl, writes=[C.t_const])
    vms = S.sb([2 * NSW], F32); ams = S.sb([2 * NSW], F32)
    S.dma('sp', vms, vmb, writes=[C.t_const]); S.dma('sp', ams, amb, writes=[C.t_const])
    bandc = [S.sb([WC_], BF16) for _ in range(4)]; t_bandc = [Trk() for _ in range(4)]
    for r in range(4):
        load_band(bandc[r], t_bandc[r], zc[r], WC_)
    q4 = [S.sb([4, 512], BF16) for _ in range(2)]; t_q4 = [Trk(), Trk()]
    ptc = [S.sb([512], BF16) for _ in range(NCB)]; t_ptc = [Trk() for _ in range(NCB)]
    impacc = [S.sb([NSW], F32) for _ in range(4)]; t_imp = [Trk() for _ in range(4)]
    impf = S.sb([NSW], F32); t_impf = Trk()
    imp2 = S.sb([NSW], F32); t_imp2 = Trk()
    m8a = S.sb([8], F32); m8b = S.sb([8], F32); t_m8 = Trk()
    rd1 = S.sb([1], F32); t_rd1 = Trk()
    nmq = S.sb([NSW], BF16); t_nmq = Trk()
    nmT = [S.sb([512], BF16) for _ in range(2)]; t_nmT = [Trk(), Trk()]
    ocs = [S.sb([512], F32) for _ in range(2)]; t_ocs = [Trk(), Trk()]
    oci = 0
    for s in range(NS):
        sl = s % 2
        cs = slice(s * 512, (s + 1) * 512)
        S.dma('sp', q4[sl][0:64], FMd[0:256, cs].rearrange("(r d) t -> d r t", d=64), writes=[t_q4[sl]])
        for r in range(4):
            tiles = []
            for jc in range(NCB):
                off = 512 * s - 2048 * jc
                if off < 0:
                    continue
                off = min(off, 5120)
                tiles.append(dict(kT=kcT[0:64, jc * 128:(jc + 1) * 128], t_k=t_kcT, v=vcp[:, jc, :], t_v=t_vcp,
                                  band=bandc[r][:, off:off + 512], t_band=t_bandc[r], keep=(ptc[jc], t_ptc[jc]), jc=jc))
            attn_tiles(S, C, q4[sl][0:64, r, :], t_q4[sl], tiles)
            for qb in range(4):
                for ti, tl in enumerate(tiles):
                    jc = tl['jc']
                    S.op('pe', mm(banks[4][:, 0:NSW + 1], ptc[jc][:, qb * 128:(qb + 1) * 128], ovls[:, jc, :], ti == 0, ti == len(tiles) - 1),
                         reads=[t_ptc[jc], C.t_const], writes=[t_bank[4]])
                S.op('dve', lambda e: e.tensor_scalar(out=rd1, in0=banks[4][:, NSW:NSW + 1], scalar1=1e-30, scalar2=None, op0=ALU.max), reads=[t_bank[4]], writes=[t_rd1])
                S.op('dve', lambda e: e.reciprocal(out=rd1, in_=rd1), reads=[t_rd1], writes=[t_rd1])
                if r == 0:
                    S.op('dve', lambda e, qb=qb: e.tensor_scalar(out=impacc[qb], in0=banks[4][:, 0:NSW], scalar1=rd1, scalar2=None, op0=ALU.mult),
                         reads=[t_bank[4], t_rd1], writes=[t_imp[qb]])
                else:
                    S.op('dve', lambda e, qb=qb: e.scalar_tensor_tensor(out=impacc[qb], in0=banks[4][:, 0:NSW], scalar=rd1, in1=impacc[qb], op0=ALU.mult, op1=ALU.add),
                         reads=[t_bank[4], t_rd1, t_imp[qb]], writes=[t_imp[qb]])
            ol = oci % 2; oci += 1
            attn_finish(S, C, ocs[ol][0:64], t_ocs[ol])
            S.dma('pool', OCd[r * 64:(r + 1) * 64, cs], ocs[ol][0:64], reads=[t_ocs[ol]])
        for qb in range(4):
            i = 4 * s + qb
            st0 = (NSW - 2) - 2 * i
            S.op('dve', lambda e, qb=qb, st0=st0: e.tensor_tensor(out=impf, in0=impacc[qb], in1=vms[:, st0:st0 + NSW], op=ALU.mult), reads=[t_imp[qb], C.t_const], writes=[t_impf])
            S.op('dve', lambda e, st0=st0: e.tensor_tensor(out=impf, in0=impf, in1=ams[:, st0:st0 + NSW], op=ALU.add), reads=[t_impf, C.t_const], writes=[t_impf])
            S.op('dve', lambda e: e.memset(impf[:, 0:1], BIGV), reads=[t_impf], writes=[t_impf])
            S.op('dve', lambda e: e.max(out=m8a, in_=impf), reads=[t_impf], writes=[t_m8])
            S.op('dve', lambda e: e.match_replace(out=imp2, in_to_replace=m8a, in_values=impf, imm_value=-BIGV), reads=[t_impf, t_m8], writes=[t_imp2])
            S.op('dve', lambda e: e.max(out=m8b, in_=imp2), reads=[t_imp2], writes=[t_m8])
            S.op('dve', lambda e: e.tensor_scalar(out=nmq, in0=impf, scalar1=m8b[:, 7:8], scalar2=NEGM, op0=ALU.is_lt, op1=ALU.mult), reads=[t_impf, t_m8], writes=[t_nmq])
            S.op('pe', lambda e, qb=qb: e.transpose(out=pbb[0:NSW, qb * 128:(qb + 1) * 128], in_=nmq, identity=identb), reads=[t_nmq, C.t_const], writes=[t_pbb])
        S.op('dve', lambda e, sl=sl: e.tensor_copy(out=nmT[sl][0:NSW], in_=pbb[0:NSW, 0:512]), reads=[t_pbb], writes=[t_nmT[sl]])
        S.dma('pool', NMd[s, 0:NSW, :], nmT[sl][0:NSW], reads=[t_nmT[sl]])
    gsb = S.sb([T], F32); t_gsb = Trk()
    S.dma('sp', gsb[0:12], GAd, writes=[t_gsb])
    S.op('act', lambda e: e.activation(out=gsb[0:12], in_=gsb[0:12], func=AF.Sigmoid), reads=[t_gsb], writes=[t_gsb])
    S.dma('pool', GSd, gsb[0:12], reads=[t_gsb])
    S.barrier()
    _reset(S, base1)

    ohAs = S.sb([NB * 128], BF16)
    S.dma('sp', ohAs, ohA, writes=[C.t_const])
    ksT = S.sb([T], BF16); kwT = S.sb([T], BF16); t_ks = Trk(); t_kw = Trk()
    vs = S.sb([NB, 64], BF16); vw = S.sb([NB, 64], BF16); t_vs = Trk(); t_vw = Trk()
    S.dma('sp', ksT[0:64], FMd[384:448, :], writes=[t_ks])
    S.dma('sp', kwT[0:64], FMd[448:512, :], writes=[t_kw])
    load_v(vs, t_vs, 0); load_v(vw, t_vw, 64)
    qT = S.sb([T], BF16); t_qT = Trk()
    bandt = S.sb([WT_], BF16); t_bandt = Trk()
    bandw = S.sb([WW_], BF16); t_bandw = Trk()
    nmTs = [S.sb([512], BF16) for _ in range(2)]; t_nmTs = [Trk(), Trk()]
    o_s = S.sb([512], F32); o_w = S.sb([512], F32); o_c = S.sb([512], F32); t_os = Trk(); t_ow = Trk(); t_oc = Trk()
    gbs = [S.sb([512], F32) for _ in range(3)]; t_gbs = [Trk() for _ in range(3)]
    oo = [S.sb([512], BF16) for _ in range(2)]; t_oo = [Trk(), Trk()]
    ni = 0
    for r in range(4):
        S.dma('sp', qT[0:64], FMd[r * 64:(r + 1) * 64, :], writes=[t_qT])
        load_band(bandt, t_bandt, zta[r], WT_)
        load_band(bandw, t_bandw, zw[r], WW_)
        for s in range(NS):
            cs = slice(s * 512, (s + 1) * 512)
            nl = ni % 2; ni += 1
            S.dma('sp', nmTs[nl][0:NSW], NMd[s, 0:NSW, :], writes=[t_nmTs[nl]])
            for b3 in range(3):
                row = b3 * 4 + r
                S.dma('sp', gbs[b3][0:64], bass.AP(GSd.tensor, row * T + s * 512, [[0, 64], [1, 512]]), writes=[t_gbs[b3]])
            S.dma('sp', o_c[0:64], OCd[r * 64:(r + 1) * 64, cs], writes=[t_oc])
            tiles = []
            for j in range(4 * s + 4):
                off = min(512 * s - 128 * j, 3072) + 384
                tiles.append(dict(kT=ksT[0:64, j * 128:(j + 1) * 128], t_k=t_ks, v=vs[:, j, :], t_v=t_vs,
                                  extra=(ohAs[0:NSW, j * 128:(j + 1) * 128], nmTs[nl][0:NSW], t_nmTs[nl]),
                                  band=bandt[:, off:off + 512], t_band=t_bandt))
            attn_tiles(S, C, qT[0:64, cs], t_qT, tiles)
            attn_finish(S, C, o_s[0:64], t_os)
            tiles = []
            for j in range(max(0, 4 * s - 4), 4 * s + 4):
                off = (512 * s - 128 * j) + 384
                tiles.append(dict(kT=kwT[0:64, j * 128:(j + 1) * 128], t_k=t_kw, v=vw[:, j, :], t_v=t_vw,
                                  band=bandw[:, off:off + 512], t_band=t_bandw))
            attn_tiles(S, C, qT[0:64, cs], t_qT, tiles)
            attn_finish(S, C, o_w[0:64], t_ow)
            ol = (r * NS + s) % 2
            S.op('pool', lambda e: e.tensor_tensor(out=o_c[0:64], in0=o_c[0:64], in1=gbs[0][0:64], op=ALU.mult), reads=[t_oc, t_gbs[0]], writes=[t_oc])
            S.op('pool', lambda e: e.tensor_tensor(out=o_s[0:64], in0=o_s[0:64], in1=gbs[1][0:64], op=ALU.mult), reads=[t_os, t_gbs[1]], writes=[t_os])
            S.op('dve', lambda e: e.tensor_tensor(out=o_w[0:64], in0=o_w[0:64], in1=gbs[2][0:64], op=ALU.mult), reads=[t_ow, t_gbs[2]], writes=[t_ow])
            S.op('pool', lambda e: e.tensor_tensor(out=o_c[0:64], in0=o_c[0:64], in1=o_s[0:64], op=ALU.add), reads=[t_oc, t_os], writes=[t_oc])
            S.op('dve', lambda e, ol=ol: e.tensor_tensor(out=oo[ol][0:64], in0=o_c[0:64], in1=o_w[0:64], op=ALU.add), reads=[t_oc, t_ow], writes=[t_oo[ol]])
            out_evs.append(S.dma('pool', oT[r * 64:(r + 1) * 64, cs], oo[ol][0:64], reads=[t_oo[ol]]))
    S.barrier()
    _reset(S, base1)

    qT = S.sb([T], BF16); t_qT = Trk()
    kT = S.sb([T], BF16); t_kT = Trk()
    vv = S.sb([NB, 64], BF16); t_vv = Trk()
    bandt = S.sb([WT_], BF16); t_bandt = Trk()
    ohBs = S.sb([32 * 128], BF16)
    S.dma('sp', ohBs[0:32], ohB, writes=[C.t_const])
    cds = S.sb([WD_], BF16); t_cds = Trk()
    S.dma('sp', cds, cd, writes=[t_cds])
    ccs = S.sb([896], BF16); t_ccs = Trk()
    S.dma('sp', ccs, ccm, writes=[t_ccs])
    negcc = S.sb([896], BF16)
    S.op('dve', lambda e: e.tensor_scalar(out=negcc, in0=ccs, scalar1=1.0, scalar2=-NEGM, op0=ALU.subtract, op1=ALU.mult), reads=[t_ccs], writes=[t_ccs])
    GW = max(NKB, 8)
    kmf = S.sb([NKB], F32); kmT = S.sb([NKB], BF16); t_km = Trk()
    gm = S.sb([GW], F32); t_gm = Trk()
    m8B = S.sb([8], F32); t_m8 = Trk()
    nmqB = S.sb([GW], BF16); t_nmq = Trk()
    nmTB = [S.sb([512], BF16) for _ in range(2)]; t_nmT = [Trk(), Trk()]
    ooB = [S.sb([512], BF16) for _ in range(2)]; t_oo = [Trk(), Trk()]
    oi = 0
    for h in range(4):
        S.dma('sp', qT[0:64], FMd[512 + h * 64:512 + (h + 1) * 64, :], writes=[t_qT])
        S.dma('sp', kT[0:64], FMd[768 + h * 64:768 + (h + 1) * 64, :], writes=[t_kT])
        load_v(vv, t_vv, 128 + h * 64)
        load_band(bandt, t_bandt, ztb[h], WT_)
        S.op('dve', lambda e: e.tensor_reduce(out=kmf[0:64], in_=kT[0:64].rearrange("p (n k) -> p n k", k=256), axis=AX.X, op=ALU.add), reads=[t_kT], writes=[t_km])
        S.op('dve', lambda e: e.tensor_scalar(out=kmT[0:64], in0=kmf[0:64], scalar1=1.0 / 256, scalar2=None, op0=ALU.mult), reads=[t_km], writes=[t_km])
        for s in range(NS):
            cs = slice(s * 512, (s + 1) * 512)
            nl = (h * NS + s) % 2
            for qb in range(4):
                i = 4 * s + qb
                cur = i // 2
                S.op('pe', mm(banks[4][:, 0:NKB], qT[0:64, i * 128:(i + 1) * 128], kmT[0:64, 0:NKB], True, True), reads=[t_qT, t_km], writes=[t_bank[4]])
                S.op('pool', lambda e: e.memset(gm, -BIGV), writes=[t_gm])
                if cur > 0:
                    S.op('dve', lambda e, cur=cur: e.tensor_copy(out=gm[:, 0:cur], in_=banks[4][:, 0:cur]), reads=[t_bank[4], t_gm], writes=[t_gm])
                S.op('dve', lambda e: e.max(out=m8B, in_=gm), reads=[t_gm], writes=[t_m8])
                S.op('dve', lambda e, cur=cur: e.memset(gm[:, cur:cur + 1], BIGV), reads=[t_gm], writes=[t_gm])
                S.op('dve', lambda e: e.tensor_scalar(out=nmqB, in0=gm, scalar1=m8B[:, 2:3], scalar2=NEGM, op0=ALU.is_lt, op1=ALU.mult), reads=[t_gm, t_m8], writes=[t_nmq])
                S.op('pe', lambda e, qb=qb: e.transpose(out=pbb[0:GW, qb * 128:(qb + 1) * 128], in_=nmqB, identity=identb), reads=[t_nmq, C.t_const], writes=[t_pbb])
            S.op('dve', lambda e, nl=nl: e.tensor_copy(out=nmTB[nl][0:GW], in_=pbb[0:GW, 0:512]), reads=[t_pbb], writes=[t_nmT[nl]])
            tiles = []
            for j in range(4 * s + 4):
                off = min(512 * s - 128 * j, 3072) + 384
                n = j // 2
                tiles.append(dict(kT=kT[0:64, j * 128:(j + 1) * 128], t_k=t_kT, v=vv[:, j, :], t_v=t_vv,
                                  extra=(ohBs[0:NKB, n * 128:(n + 1) * 128], nmTB[nl][0:NKB], t_nmT[nl]),
                                  band=bandt[:, off:off + 512], t_band=t_bandt))
            attn_tiles(S, C, qT[0:64, cs], t_qT, tiles)
            ol = oi % 2; oi += 1
            attn_finish(S, C, ooB[ol][0:64], t_oo[ol])
            out_evs.append(S.dma('pool', oT[256 + h * 64:256 + (h + 1) * 64, cs], ooB[ol][0:64], reads=[t_oo[ol]]))
    for h in range(4):
        S.dma('sp', qT[0:64], FMd[1536 + h * 64:1536 + (h + 1) * 64, :], writes=[t_qT])
        S.dma('sp', kT[0:64], FMd[1792 + h * 64:1792 + (h + 1) * 64, :], writes=[t_kT])
        load_v(vv, t_vv, 640 + h * 64)
        load_band(bandt, t_bandt, zd[h], WD_, mult=(cds, t_cds))
        for s in range(NS):
            cs = slice(s * 512, (s + 1) * 512)
            tiles = []
            for j in range(max(0, 4 * s - 16), 4 * s + 4):
                off = (512 * s - 128 * j) + 384
                tiles.append(dict(kT=kT[0:64, j * 128:(j + 1) * 128], t_k=t_kT, v=vv[:, j, :], t_v=t_vv,
                                  band=bandt[:, off:off + 512], t_band=t_bandt))
            attn_tiles(S, C, qT[0:64, cs], t_qT, tiles)
            ol = oi % 2; oi += 1
            attn_finish(S, C, ooB[ol][0:64], t_oo[ol])
            out_evs.append(S.dma('pool', oT[768 + h * 64:768 + (h + 1) * 64, cs], ooB[ol][0:64], reads=[t_oo[ol]]))
    m3 = _mark(S)
    NB4 = NB * 4
    lfT = S.sb([NB, 4], F32); t_lf = Trk()
    spb = S.sb([NB, 4], F32); t_sp = Trk()
    cT = S.sb([NB, 4], F32); t_cT = Trk()
    pfx = [S.sb([NB, 4], F32) for _ in range(2)]; t_pfx = [Trk(), Trk()]
    tot = S.sb([NB, 4], F32); t_tot = Trk()
    fb = S.sb([4], F32); nfb = S.sb([4], F32); t_fb = Trk()
    c0s = S.sb([NS, 4], F32); c0b = S.sb([NS, 4], F32); t_c0 = Trk()
    crelT = S.sb([NB, 4], F32); t_crel = Trk()
    crow = S.sb([T], F32); t_crow = Trk()
    chi = S.sb([T], BF16); clo = S.sb([T], BF16); t_chl = Trk()
    biasall = S.sb([NS, NB, 4], F32); t_ball = Trk()
    trif = S.sb([128], F32); r0f = S.sb([128], F32)
    S.dma('sp', trif, tri, writes=[C.t_const]); S.dma('sp', r0f, r0m, writes=[C.t_const])
    S.dma('sp', fb, foxb, writes=[t_fb])
    S.op('dve', lambda e: e.tensor_scalar(out=nfb, in0=fb, scalar1=-1.0, scalar2=None, op0=ALU.mult), reads=[t_fb], writes=[t_fb])
    for j0 in range(0, NB, 8):
        j1 = min(NB, j0 + 8)
        S.dma('sp', lfT[:, j0:j1, :], FCd[j0 * 128:j1 * 128, :].rearrange("(j p) h -> p j h", p=128), writes=[t_lf])
    for h in range(4):
        S.op('act', lambda e, h=h: e.activation(out=spb[:, :, h], in_=lfT[:, :, h], func=AF.Exp, bias=nfb[:, h:h + 1], scale=-1.0), reads=[t_lf, t_fb], writes=[t_sp])
    S.op('act', lambda e: e.activation(out=spb, in_=spb, func=AF.Ln, bias=onecol, scale=1.0), reads=[t_sp, C.t_const], writes=[t_sp])
    spf = spb.rearrange("p j h -> p (j h)")
    S.op('pe', mm(banks[4][:, 0:NB4], trif, spf, True, True), reads=[t_sp, C.t_const], writes=[t_bank[4]])
    S.op('pe', mm(banks[5][:, 0:NB4], onesf, spf, True, True), reads=[t_sp, C.t_const], writes=[t_bank[5]])
    S.op('dve', lambda e: e.tensor_copy(out=tot.rearrange("p j h -> p (j h)"), in_=banks[5][:, 0:NB4]), reads=[t_bank[5]], writes=[t_tot])
    S.op('dve', lambda e: e.tensor_copy(out=pfx[0], in_=tot), reads=[t_tot], writes=[t_pfx[0]])
    cur_ = 0
    sh = 1
    while sh < NB:
        a, b_ = pfx[cur_], pfx[1 - cur_]
        S.op('dve', lambda e, a=a, b_=b_, sh=sh: e.tensor_copy(out=b_[:, 0:sh, :], in_=a[:, 0:sh, :]), reads=[t_pfx[cur_]], writes=[t_pfx[1 - cur_]])
        S.op('dve', lambda e, a=a, b_=b_, sh=sh: e.tensor_tensor(out=b_[:, sh:NB, :], in0=a[:, sh:NB, :], in1=a[:, 0:NB - sh, :], op=ALU.add), reads=[t_pfx[cur_]], writes=[t_pfx[1 - cur_]])
        cur_ = 1 - cur_
        sh *= 2
    incl = pfx[cur_]
    S.op('dve', lambda e: e.tensor_tensor(out=cT.rearrange("p j h -> p (j h)"), in0=banks[4][:, 0:NB4], in1=incl.rearrange("p j h -> p (j h)"), op=ALU.add), reads=[t_bank[4], t_pfx[cur_]], writes=[t_cT])
    S.op('dve', lambda e: e.tensor_tensor(out=cT, in0=tot, in1=cT, op=ALU.subtract), reads=[t_cT, t_tot], writes=[t_cT])
    for s in range(NS):
        S.op('dve', lambda e, s=s: e.tensor_copy(out=c0s[:, s, :], in_=cT[:, 4 * s, :]), reads=[t_cT], writes=[t_c0])
    S.op('pe', mm(banks[4][:, 0:NS * 4], r0f, c0s.rearrange("p s h -> p (s h)"), True, True), reads=[t_c0, C.t_const], writes=[t_bank[4]])
    S.op('dve', lambda e: e.tensor_copy(out=c0b.rearrange("p s h -> p (s h)"), in_=banks[4][:, 0:NS * 4]), reads=[t_bank[4]], writes=[t_c0])
    for j in range(NB):
        S.op('dve', lambda e, j=j: e.tensor_tensor(out=crelT[:, j, :], in0=cT[:, j, :], in1=c0b[:, j // 4, :], op=ALU.subtract), reads=[t_cT, t_c0], writes=[t_crel])
    for s in range(NS):
        for jj in range(4):
            j = 4 * s + jj
            S.op('pe', lambda e, j=j, jj=jj: e.transpose(out=banks[5][0:4, jj * 128:(jj + 1) * 128], in_=crelT[:, j, :], identity=identfs), reads=[t_crel, C.t_const], writes=[t_bank[5]])
        S.op('dve', lambda e, s=s: e.tensor_copy(out=crow[0:4, s * 512:(s + 1) * 512], in_=banks[5][0:4, 0:512]), reads=[t_bank[5]], writes=[t_crow])
    S.op('dve', lambda e: e.tensor_copy(out=chi[0:4], in_=crow[0:4]), reads=[t_crow], writes=[t_chl])
    S.op('dve', lambda e: e.tensor_tensor(out=crow[0:4], in0=crow[0:4], in1=chi[0:4], op=ALU.subtract), reads=[t_crow, t_chl], writes=[t_crow])
    S.op('dve', lambda e: e.tensor_copy(out=clo[0:4], in_=crow[0:4]), reads=[t_crow], writes=[t_chl])
    for s in range(NS):
        for h in range(4):
            nj = 4 * s + 4
            S.op('dve', lambda e, s=s, h=h, nj=nj: e.tensor_scalar(out=biasall[:, s, 0:nj, h], in0=cT[:, 0:nj, h], scalar1=-1.0, scalar2=c0b[:, s, h:h + 1], op0=ALU.mult, op1=ALU.add),
                 reads=[t_cT, t_c0], writes=[t_ball])
    for h in range(4):
        S.dma('sp', qT[0:64], FMd[1024 + h * 64:1024 + (h + 1) * 64, :], writes=[t_qT])
        S.dma('sp', qT[64:65], chi[h:h + 1], reads=[t_chl], writes=[t_qT])
        S.dma('sp', qT[65:66], clo[h:h + 1], reads=[t_chl], writes=[t_qT])
        S.dma('sp', kT[0:64], FMd[1280 + h * 64:1280 + (h + 1) * 64, :], writes=[t_kT])
        S.op('pool', lambda e: e.memset(kT[64:66], 1.0), writes=[t_kT])
        load_v(vv, t_vv, 384 + h * 64)
        for s in range(NS):
            cs = slice(s * 512, (s + 1) * 512)
            tiles = []
            for j in range(4 * s + 4):
                d = dict(kT=kT[0:66, j * 128:(j + 1) * 128], t_k=t_kT, v=vv[:, j, :], t_v=t_vv,
                         bias=biasall[:, s, j, h:h + 1], t_bias=t_ball)
                if j >= 4 * s:
                    off = (512 * s - 128 * j) + 384
                    d['extra'] = (identb, negcc[:, off:off + 512], t_ccs)
                tiles.append(d)
            attn_tiles(S, C, qT[0:66, cs], t_qT, tiles)
            ol = oi % 2; oi += 1
            attn_finish(S, C, ooB[ol][0:64], t_oo[ol])
            out_evs.append(S.dma('pool', oT[512 + h * 64:512 + (h + 1) * 64, cs], ooB[ol][0:64], reads=[t_oo[ol]]))
    S.emit()
    return nc


def build_B(NT):
    NS = NT // 512
    nc = bass.Bass("TRN2", target_bir_lowering=False)

    def din(n, sh, dt=F32):
        return nc.dram_tensor(n, list(sh), dt, kind="ExternalInput").ap()
    oTin = din("oTin", [2048, NT], BF16); xTin = din("xTin", [2048, NT])
    wout = din("wout", [2048, 2048]); wup = din("wup", [2048, 8192]); wdn = din("wdn", [8192, 2048])
    gpost = din("gpost", [128, 16]); gmp = din("gmp", [128, 16]); gmq = din("gmq", [128, 16])
    xo = nc.dram_tensor("xo", [2048, NT], F32, kind="ExternalOutput").ap()
    S = Sched(nc)
    S.init_arena(200 * 1024)
    banks = [S.psum("pb%d" % i) for i in range(8)]
    t_bank = [Trk() for _ in range(8)]
    t_c = Trk()
    ones_bf = S.sb([128], BF16); epscol = S.sb([1], F32)
    S.op('pool', lambda e: e.memset(ones_bf, 1.0), writes=[t_c])
    S.op('pool', lambda e: e.memset(epscol, 1e-6), writes=[t_c])
    g1 = S.sb([16], F32); g2 = S.sb([16], F32); g3 = S.sb([16], F32)
    S.dma('sp', g1, gpost, writes=[t_c]); S.dma('sp', g2, gmp, writes=[t_c]); S.dma('sp', g3, gmq, writes=[t_c])
    bufA = S.sb([16, 512], F32); t_A = Trk()
    bufX = S.sb([16, 512], F32); t_X = Trk()
    bufH = S.sb([16, 512], BF16); t_H = Trk()
    aT = S.sb([64, 512], BF16); t_aT = Trk()
    sqt = [S.sb([512], BF16) for _ in range(4)]; t_sqt = [Trk() for _ in range(4)]
    rbs = [S.sb([512], F32) for _ in range(3)]; t_rbs = [Trk() for _ in range(3)]
    wst = [S.sb([2048], F32) for _ in range(2)]; t_wst = [Trk(), Trk()]
    wbf = [S.sb([2048], BF16) for _ in range(2)]; t_wbf = [Trk(), Trk()]
    tmpf = [S.sb([512], F32) for _ in range(2)]; t_tmpf = [Trk(), Trk()]
    st = Ctx(); st.wi = 0; st.sqi = 0; st.ti = 0
    out_evs = []

    def load_w(src_ap, shape3):
        sl = st.wi % 2; st.wi += 1
        a, b = shape3
        v32 = wst[sl].rearrange("p (a b) -> p a b", b=b)
        v16 = wbf[sl].rearrange("p (a b) -> p a b", b=b)
        S.dma('sp' if sl == 0 else 'act', v32, src_ap, writes=[t_wst[sl]])
        S.op('pool', lambda e: e.tensor_copy(out=wbf[sl], in_=wst[sl]), reads=[t_wst[sl]], writes=[t_wbf[sl]])
        return v16, t_wbf[sl]

    def ss_accum(bank, t_bk, src_ap, t_src, first, last, from_psum_t=None):
        q = st.sqi % 4; st.sqi += 1
        S.op('act', lambda e: e.activation(out=sqt[q], in_=src_ap, func=AF.Square), reads=[t_src], writes=[t_sqt[q]])
        S.op('pe', mm(bank, ones_bf, sqt[q], first, last), reads=[t_sqt[q], t_c], writes=[t_bk])

    for s in range(NS):
        cs = slice(s * 512, (s + 1) * 512)
        S.dma('sp', bufH, oTin[:, cs].rearrange("(c p) t -> p c t", p=128), writes=[t_H])
        S.dma('act', bufX, xTin[:, cs].rearrange("(c p) t -> p c t", p=128), writes=[t_X])
        for dc in range(16):
            wv, t_w = load_w(wout[:, dc * 128:(dc + 1) * 128].rearrange("(c p) m -> p c m", p=128), (16, 128))
            bk = dc % 4
            for kc in range(16):
                S.op('pe', mm(banks[bk], wv[:, kc, :], bufH[:, kc, :], kc == 0, kc == 15), reads=[t_w, t_H], writes=[t_bank[bk]])
            S.op('dve', lambda e, dc=dc, bk=bk: e.tensor_copy(out=bufA[:, dc, :], in_=banks[bk]), reads=[t_bank[bk]], writes=[t_A])
            ss_accum(banks[4], t_bank[4], bufA[:, dc, :], t_A, dc == 0, dc == 15)
        rstd_from_ps(S, banks[4], rbs[0], t_bank[4], t_rbs[0], epscol, t_c, 1.0 / 2048)
        for dc in range(16):
            S.op('dve', lambda e, dc=dc: e.scalar_tensor_tensor(out=bufA[:, dc, :], in0=bufA[:, dc, :], scalar=g1[:, dc:dc + 1], in1=rbs[0], op0=ALU.mult, op1=ALU.mult),
                 reads=[t_A, t_rbs[0], t_c], writes=[t_A])
            S.op('pool', lambda e, dc=dc: e.tensor_tensor(out=bufX[:, dc, :], in0=bufX[:, dc, :], in1=bufA[:, dc, :], op=ALU.add), reads=[t_A, t_X], writes=[t_X])
            S.op('dve', lambda e, dc=dc: e.tensor_scalar(out=bufH[:, dc, :], in0=bufX[:, dc, :], scalar1=g2[:, dc:dc + 1], scalar2=None, op0=ALU.mult), reads=[t_X, t_c], writes=[t_H])
            ss_accum(banks[5], t_bank[5], bufX[:, dc, :], t_X, dc == 0, dc == 15)
        rstd_from_ps(S, banks[5], rbs[1], t_bank[5], t_rbs[1], epscol, t_c, 1.0 / 2048)
        for f in range(64):
            wv, t_w = load_w(wup[:, f * 128:(f + 1) * 128].rearrange("(c p) m -> p c m", p=128), (16, 128))
            bk = f % 4
            for kc in range(16):
                S.op('pe', mm(banks[bk], wv[:, kc, :], bufH[:, kc, :], kc == 0, kc == 15), reads=[t_w, t_H], writes=[t_bank[bk]])
            tq = st.ti % 2; st.ti += 1
            S.op('dve', lambda e, bk=bk, tq=tq: e.scalar_tensor_tensor(out=tmpf[tq], in0=banks[bk], scalar=0.0, in1=rbs[1], op0=ALU.max, op1=ALU.mult),
                 reads=[t_bank[bk], t_rbs[1]], writes=[t_tmpf[tq]])
            S.op('act', lambda e, f=f, tq=tq: e.activation(out=aT[:, f, :], in_=tmpf[tq], func=AF.Square), reads=[t_tmpf[tq]], writes=[t_aT])
        for dg in range(4):
            for fg in range(16):
                wv, t_w = load_w(wdn[fg * 512:(fg + 1) * 512, dg * 512:(dg + 1) * 512].rearrange("(c p) m -> p c m", p=128), (4, 512))
                for c4 in range(4):
                    f = fg * 4 + c4
                    for d4 in range(4):
                        S.op('pe', mm(banks[d4], wv[:, c4, d4 * 128:(d4 + 1) * 128], aT[:, f, :], f == 0, f == 63), reads=[t_w, t_aT], writes=[t_bank[d4]])
            for d4 in range(4):
                dc = dg * 4 + d4
                S.op('dve', lambda e, dc=dc, d4=d4: e.tensor_copy(out=bufA[:, dc, :], in_=banks[d4]), reads=[t_bank[d4]], writes=[t_A])
                ss_accum(banks[6], t_bank[6], bufA[:, dc, :], t_A, dc == 0, dc == 15)
        rstd_from_ps(S, banks[6], rbs[2], t_bank[6], t_rbs[2], epscol, t_c, 1.0 / 2048)
        for dc in range(16):
            S.op('dve', lambda e, dc=dc: e.scalar_tensor_tensor(out=bufA[:, dc, :], in0=bufA[:, dc, :], scalar=g3[:, dc:dc + 1], in1=rbs[2], op0=ALU.mult, op1=ALU.mult),
                 reads=[t_A, t_rbs[2], t_c], writes=[t_A])
            S.op('pool', lambda e, dc=dc: e.tensor_tensor(out=bufA[:, dc, :], in0=bufA[:, dc, :], in1=bufX[:, dc, :], op=ALU.add), reads=[t_A, t_X], writes=[t_A])
        out_evs.append(S.dma('pool', xo[:, cs].rearrange("(c p) t -> p c t", p=128), bufA, reads=[t_A]))
    S.emit()
    return nc


def _t5_bucket_np(d):
    d = np.maximum(d, 0)
    rel = np.log(np.maximum(d, 1).astype(np.float32) / np.float32(16)) / np.float32(math.log(4096 / 16))
    large = np.minimum(16 + (rel * np.float32(16)).astype(np.int32), 31)
    return np.where(d < 16, d, large).astype(np.int64)


def _consts(T):
    NB, NSW = T // 128, T // 64
    NCB = max(1, T // 2048)
    NCMP = T // 16 - 1
    k = np.arange(128)[:, None]
    c = {}
    c['ident'] = np.eye(128, dtype=np.float32).astype(BFNP)
    c['identf'] = np.eye(128, dtype=np.float32)
    jk = np.arange(NB * 128)[None, :]
    c['ohA'] = (k == 2 * (jk // 128) + (jk % 128) // 64).astype(np.float32).astype(BFNP)
    n32 = np.arange(32)[:, None]
    c['ohB'] = (n32 == (np.arange(32 * 128)[None, :] // 128)).astype(np.float32).astype(BFNP)
    ov = np.zeros((128, NCB, NSW + 1), np.float32)
    for jc in range(NCB):
        cc = jc * 128 + np.arange(128)
        n = np.arange(NSW)[None, :]
        m = ((16 * cc[:, None]) < 64 * n + 64) & ((16 * cc[:, None] + 31) >= 64 * n) & (cc[:, None] < NCMP)
        ov[:, jc, :NSW] = m
        ov[:, jc, NSW] = 1.0
    c['ovl'] = ov.astype(BFNP)
    m = np.arange(2 * NSW)[None, :]
    npr = m - (NSW - 2)
    curp = (k >= 64).astype(np.int64)
    valid = npr <= curp
    forced = (npr == curp) | (npr == curp - 1)
    c['vmb'] = (valid & ~forced).astype(np.float32)
    c['amb'] = np.where(forced, np.float32(BIGV), np.where(valid, np.float32(0), np.float32(-BIGV))).astype(np.float32)
    c['tri'] = (k <= np.arange(128)[None, :]).astype(np.float32)
    r0 = np.zeros((128, 128), np.float32); r0[0, :] = 1.0
    c['r0m'] = r0
    dist_t = np.arange(WT_)[None, :] - 384 - k
    dist_w = np.arange(WW_)[None, :] - 384 - k
    dist_c = np.arange(WC_)[None, :] - 31 - 16 * k
    dist_d = np.arange(WD_)[None, :] - 384 - k
    mult = (((dist_d >= 0) & (dist_d <= 128)).astype(np.int64)
            + ((dist_d >= 0) & (dist_d <= 512) & (dist_d % 4 == 0))
            + ((dist_d >= 0) & (dist_d <= 2048) & (dist_d % 16 == 0)))
    c['cd'] = mult.astype(np.float32).astype(BFNP)
    c['ccm'] = ((np.arange(896)[None, :] - 384 - k) >= 0).astype(np.float32).astype(BFNP)
    c['_maps'] = dict(t=(dist_t, dist_t >= 0), w=(dist_w, (dist_w >= 0) & (dist_w <= 511)),
                      c=(dist_c, dist_c >= 0), d=(dist_d, mult > 0))
    return c


def _band(tabh, dist, valid):
    idx = _t5_bucket_np(np.clip(dist, 0, None))
    out = tabh[idx].astype(np.float32)
    out[~valid] = np.float32(NEGM)
    return out


_PROJ_OFF = dict(qa=0, kca=512, vca=640, ksa=768, vsa=896, kwa=1024, vwa=1152, ga=1280, qb=1304, kb=1816, vb=2328,
                 qc=2840, kc=3352, vc=3864, fc=4376, qd=4384, kd=4896, vd=5408)


def _cols(g):
    P = _PROJ_OFF
    r256 = np.arange(256)
    r64 = np.arange(64)
    fm = [P['qa'] + g * 256 + r256, P['kca'] + g * 64 + r64, P['vca'] + g * 64 + r64, P['ksa'] + g * 64 + r64, P['kwa'] + g * 64 + r64,
          P['qb'] + g * 256 + r256, P['kb'] + g * 256 + r256, P['qc'] + g * 256 + r256, P['kc'] + g * 256 + r256,
          P['qd'] + g * 256 + r256, P['kd'] + g * 256 + r256,
          np.array([P['ga'] + b3 * 8 + g * 4 + r for b3 in range(3) for r in range(4)])]
    tm = [P['vsa'] + g * 64 + r64, P['vwa'] + g * 64 + r64, P['vb'] + g * 256 + r256, P['vc'] + g * 256 + r256, P['vd'] + g * 256 + r256,
          P['fc'] + g * 4 + np.arange(4)]
    return np.concatenate(fm + tm)


def _pm(v):
    return np.ascontiguousarray(np.asarray(v, np.float32).reshape(16, 128).T)


_NC_CACHE = {}
_DBG = False


def _run_model(inp, B, T, depth):
    f32 = lambda a: np.asarray(a, dtype=np.float32)
    x = f32(inp['x'])
    rel_bias = f32(inp['rel_bias'])
    cst = _consts(T)
    maps = cst.pop('_maps')
    ncores = 2 * B
    if ('A', T) not in _NC_CACHE:
        _NC_CACHE[('A', T)] = build_A(T)
        _NC_CACHE[('B', T)] = build_B(T // 2)
    ncA, ncB = _NC_CACHE[('A', T)], _NC_CACHE[('B', T)]
    bands = []
    for g in range(2):
        d = {}
        d['zta'] = np.stack([_band(rel_bias[:, 4 * g + r], *maps['t']) for r in range(4)])
        d['zw'] = np.stack([_band(rel_bias[:, 4 * g + r], *maps['w']) for r in range(4)])
        d['zc'] = np.stack([_band(rel_bias[:, 4 * g + r], *maps['c']) for r in range(4)])
        d['ztb'] = np.stack([_band(rel_bias[:, 8 + 4 * g + r], *maps['t']) for r in range(4)])
        d['zd'] = np.stack([_band(rel_bias[:, 16 + 4 * g + r], *maps['d']) for r in range(4)])
        bands.append(d)
    xT = [np.ascontiguousarray(x[b].T) for b in range(B)]
    H = T // 2
    for l in range(depth):
        w_in = f32(inp['w_in'][l])
        in_maps = []
        for c in range(ncores):
            b, g = c // 2, c % 2
            m = dict(cst)
            m.update(bands[g])
            m['xT'] = xT[b]
            m['w'] = np.ascontiguousarray(w_in[:, _cols(g)])
            m['gpre'] = _pm(inp['g_mix_pre'][l])
            m['foxb'] = np.ascontiguousarray(np.broadcast_to(f32(inp['fox_bias'][l])[4 * g:4 * g + 4][None, :], (128, 4)))
            m['w1k'] = f32(inp['phik_w1'][l]); m['w2k'] = f32(inp['phik_w2'][l])
            m['w1v'] = f32(inp['phiv_w1'][l]); m['w2v'] = f32(inp['phiv_w2'][l])
            m['peT'] = np.ascontiguousarray(f32(inp['cmp_pe'][l]).T)
            in_maps.append(m)
        resA = run_bass_kernel_spmd(ncA, in_maps, core_ids=list(range(ncores)))
        oTs = [np.asarray(r['oT']) for r in resA.results]
        if _DBG:
            print('L%d A finite' % l, [bool(np.isfinite(o.astype(np.float32)).all()) for o in oTs], flush=True)
        in_maps = []
        for c in range(ncores):
            b, hf = c // 2, c % 2
            o_full = np.empty((2048, H), dtype=oTs[0].dtype)
            for mixer in range(4):
                for g in range(2):
                    o_full[mixer * 512 + g * 256:mixer * 512 + (g + 1) * 256, :] = oTs[2 * b + g][mixer * 256:(mixer + 1) * 256, hf * H:(hf + 1) * H]
            in_maps.append(dict(oTin=o_full, xTin=np.ascontiguousarray(xT[b][:, hf * H:(hf + 1) * H]),
                                wout=f32(inp['w_out'][l]), wup=f32(inp['w_up'][l]), wdn=f32(inp['w_down'][l]),
                                gpost=_pm(inp['g_mix_post'][l]), gmp=_pm(inp['g_mlp_pre'][l]), gmq=_pm(inp['g_mlp_post'][l])))
        resB = run_bass_kernel_spmd(ncB, in_maps, core_ids=list(range(ncores)))
        for c in range(ncores):
            b, hf = c // 2, c % 2
            xT[b][:, hf * H:(hf + 1) * H] = np.asarray(resB.results[c]['xo'])
            if _DBG:
                xo_ = np.asarray(resB.results[c]['xo'])
                bad = ~np.isfinite(xo_)
                cols = np.where(bad.any(axis=0))[0]
                print('L%d B core %d nbad' % (l, c), int(bad.sum()), (int(cols.min()), int(cols.max()), len(cols)) if len(cols) else None, float(np.nanmax(np.abs(xo_))), flush=True)
    return np.stack([np.ascontiguousarray(xT[b].T) for b in range(B)]).astype(np.float32)


def kernel(x, w_in, w_out, g_mix_pre, g_mix_post, g_mlp_pre, g_mlp_post, w_up, w_down,
           cmp_pe, phik_w1, phik_w2, phiv_w1, phiv_w2, fox_bias, rel_bias):
    inp = dict(x=x, w_in=w_in, w_out=w_out, g_mix_pre=g_mix_pre, g_mix_post=g_mix_post, g_mlp_pre=g_mlp_pre,
               g_mlp_post=g_mlp_post, w_up=w_up, w_down=w_down, cmp_pe=cmp_pe, phik_w1=phik_w1, phik_w2=phik_w2,
               phiv_w1=phiv_w1, phiv_w2=phiv_w2, fox_bias=fox_bias, rel_bias=rel_bias)
    xs = np.asarray(x)
    return _run_model(inp, xs.shape[0], xs.shape[1], np.asarray(w_in).shape[0])
```
